# Optimizing a Trainium2 kernel written in Bass

```python
import math
import jax, jax.numpy as jnp
from jax import lax
import numpy as np

D_MODEL = 2048
BATCH = 1
SEQ = 8192
DEPTH = 4

GRID_W = 64
CTX_LEN = 256
ROPE_THETA = 10000.0
ROPE_DIM = 64
Q_BLOCK = 128
NEG_INF = -1e30
EPS = 1e-6

A_HEADS = 4
A_QK_DIM = 64
A_V_DIM = 2 * A_QK_DIM
B_HEADS = 4
B_Q_LORA = 512
B_KV_LORA = 256
B_NOPE = 128
B_ROPE = 64
B_V = 128
C_HEADS = 8
C_KV_HEADS = 2
C_GROUP = C_HEADS // C_KV_HEADS
C_HEAD_DIM = 64
C_WINDOW = 128
C_BLOCK = 128
D_HEADS = 4
D_HEAD_DIM = 128
NA_ROWS = 8
NA_COLS = 16

N_BRANCHES = 4
BRANCH_WIDTH = 512
A_IN = A_HEADS * (4 * A_QK_DIM + A_V_DIM)
B_IN = B_Q_LORA + B_KV_LORA + B_ROPE
C_IN = (C_HEADS + 2 * C_KV_HEADS) * C_HEAD_DIM
D_IN = 3 * D_HEADS * D_HEAD_DIM
IN_WIDTH = A_IN + B_IN + C_IN + D_IN
IN_SPLITS = (A_IN, A_IN + B_IN, A_IN + B_IN + C_IN)

PEER_HEADS = 8
PEER_N_KEYS = 128
PEER_N_EXPERTS = PEER_N_KEYS ** 2
PEER_KEY_DIM = 256
PEER_TOPK = 16
PEER_BLOCK = 128

kernel_name = 'hybrid_gated_mixers_peer_dit'


def rms_norm(x, g):
    xf = x.astype(jnp.float32)
    y = xf * lax.rsqrt(jnp.mean(xf * xf, axis=-1, keepdims=True) + EPS)
    return (y * g.astype(jnp.float32)).astype(x.dtype)


def modulate(h, shift, scale):
    return h * (1.0 + scale) + shift


def axial_rope(n_tokens):
    t = jnp.arange(n_tokens, dtype=jnp.int32)
    row = (t // GRID_W).astype(jnp.float32)
    col = (t % GRID_W).astype(jnp.float32)
    n_freq = ROPE_DIM // 4
    inv = ROPE_THETA ** (-jnp.arange(n_freq, dtype=jnp.float32) / n_freq)
    ang = jnp.concatenate([row[:, None] * inv, col[:, None] * inv], axis=-1)
    return jnp.cos(ang), jnp.sin(ang)


def apply_rope(x, cos, sin):
    shape = (1, cos.shape[0]) + (1,) * (x.ndim - 3) + (cos.shape[1],)
    cs, sn = cos.reshape(shape), sin.reshape(shape)
    x1, x2 = jnp.split(x.astype(jnp.float32), 2, axis=-1)
    return jnp.concatenate([x1 * cs - x2 * sn, x1 * sn + x2 * cs], axis=-1).astype(x.dtype)


def softmax_attend(q, k, v, scale):
    s = jnp.einsum('bqhd,bkhd->bhqk', q, k).astype(jnp.float32) * scale
    p = jax.nn.softmax(s, axis=-1).astype(v.dtype)
    return jnp.einsum('bhqk,bkhd->bqhd', p, v)


def softmax_with_sink(s, sink):
    sink = jnp.broadcast_to(sink, s.shape[:-1] + (1,))
    p = jax.nn.softmax(jnp.concatenate([s, sink], axis=-1), axis=-1)
    return p[..., :-1]


def sweep_query_blocks(attend, q):
    b, n = q.shape[:2]
    nb = n // Q_BLOCK
    qb = jnp.moveaxis(q.reshape((b, nb, Q_BLOCK) + q.shape[2:]), 1, 0)
    o = lax.map(attend, qb)
    return jnp.moveaxis(o, 0, 1).reshape((b, n) + o.shape[3:])


def diff_attention(pa_lat, pa_ctx, cos, sin, lam_q1, lam_k1, lam_q2, lam_k2, subln_g, lam_init, with_ctx):
    def heads(p):
        b, n = p.shape[:2]
        q, k, v = jnp.split(p, (2 * A_HEADS * A_QK_DIM, 4 * A_HEADS * A_QK_DIM), axis=-1)
        return (q.reshape(b, n, A_HEADS, 2, A_QK_DIM), k.reshape(b, n, A_HEADS, 2, A_QK_DIM),
                v.reshape(b, n, A_HEADS, A_V_DIM))
    ql, kl, vl = heads(pa_lat)
    qc, kc, vc = heads(pa_ctx)
    ql, kl = apply_rope(ql, cos, sin), apply_rope(kl, cos, sin)
    f32 = jnp.float32
    lam = (jnp.exp(jnp.sum(lam_q1.astype(f32) * lam_k1.astype(f32)))
           - jnp.exp(jnp.sum(lam_q2.astype(f32) * lam_k2.astype(f32))) + lam_init)
    scale = A_QK_DIM ** -0.5

    def attend(q, k, v):
        s = jnp.einsum('bqhmd,bkhmd->bhmqk', q, k).astype(f32) * scale
        p = jax.nn.softmax(s, axis=-1)
        a = (p[:, :, 0] - lam * p[:, :, 1]).astype(v.dtype)
        return jnp.einsum('bhqk,bkhd->bqhd', a, v)

    def finish(o):
        b, n = o.shape[:2]
        return (rms_norm(o, subln_g) * (1.0 - lam_init)).reshape(b, n, A_HEADS * A_V_DIM)

    k_all = jnp.concatenate([kl, kc], axis=1)
    v_all = jnp.concatenate([vl, vc], axis=1)
    o_lat = finish(sweep_query_blocks(lambda qb: attend(qb, k_all, v_all), ql))
    o_ctx = finish(attend(qc, kc, vc)) if with_ctx else None
    return o_lat, o_ctx


def latent_attention(pb_lat, pb_ctx, cos, sin, qa_norm, w_uq, kva_norm, w_ukv, with_ctx):
    scale = (B_NOPE + B_ROPE) ** -0.5

    def queries(p, rotate):
        b, n = p.shape[:2]
        q = (rms_norm(p[..., :B_Q_LORA], qa_norm) @ w_uq).reshape(b, n, B_HEADS, B_NOPE + B_ROPE)
        q_nope, q_pe = jnp.split(q, (B_NOPE,), axis=-1)
        if rotate:
            q_pe = apply_rope(q_pe, cos, sin)
        return jnp.concatenate([q_nope, q_pe], axis=-1)

    def keys_values(p, rotate):
        b, n = p.shape[:2]
        c_kv = rms_norm(p[..., B_Q_LORA:B_Q_LORA + B_KV_LORA], kva_norm)
        kv = (c_kv @ w_ukv).reshape(b, n, B_HEADS, B_NOPE + B_V)
        k_nope, v = jnp.split(kv, (B_NOPE,), axis=-1)
        k_pe = p[..., B_Q_LORA + B_KV_LORA:][:, :, None, :]
        if rotate:
            k_pe = apply_rope(k_pe, cos, sin)
        k = jnp.concatenate([k_nope, jnp.broadcast_to(k_pe, (b, n, B_HEADS, B_ROPE))], axis=-1)
        return k, v

    b, n = pb_lat.shape[:2]
    kl, vl = keys_values(pb_lat, True)
    kc, vc = keys_values(pb_ctx, False)
    k_all = jnp.concatenate([kl, kc], axis=1)
    v_all = jnp.concatenate([vl, vc], axis=1)
    ql = queries(pb_lat, True)
    o_lat = sweep_query_blocks(lambda qb: softmax_attend(qb, k_all, v_all, scale), ql).reshape(b, n, B_HEADS * B_V)
    o_ctx = None
    if with_ctx:
        nc = pb_ctx.shape[1]
        o_ctx = softmax_attend(queries(pb_ctx, False), kc, vc, scale).reshape(pb_ctx.shape[0], nc, B_HEADS * B_V)
    return o_lat, o_ctx


def window_attention(pc_lat, pc_ctx, cos, sin, sink, with_ctx):
    def heads(p):
        b, n = p.shape[:2]
        q, k, v = jnp.split(p, (C_HEADS * C_HEAD_DIM, (C_HEADS + C_KV_HEADS) * C_HEAD_DIM), axis=-1)
        return (q.reshape(b, n, C_KV_HEADS, C_GROUP, C_HEAD_DIM), k.reshape(b, n, C_KV_HEADS, C_HEAD_DIM),
                v.reshape(b, n, C_KV_HEADS, C_HEAD_DIM))
    f32 = jnp.float32
    ql, kl, vl = heads(pc_lat)
    qc, kc, vc = heads(pc_ctx)
    ql, kl = apply_rope(ql, cos, sin), apply_rope(kl, cos, sin)
    b, n = pc_lat.shape[:2]
    nb = n // C_BLOCK
    nk = 3 * C_BLOCK
    scale = C_HEAD_DIM ** -0.5
    sink = sink.astype(f32).reshape(C_KV_HEADS, C_GROUP, 1, 1)

    def band(t):
        tp = jnp.pad(t, ((0, 0), (C_BLOCK, C_BLOCK), (0, 0), (0, 0))).reshape(b, nb + 2, C_BLOCK, C_KV_HEADS, C_HEAD_DIM)
        return jnp.concatenate([tp[:, :-2], tp[:, 1:-1], tp[:, 2:]], axis=2)

    kb, vb = band(kl), band(vl)
    qb = ql.reshape(b, nb, C_BLOCK, C_KV_HEADS, C_GROUP, C_HEAD_DIM)
    qi = jnp.arange(C_BLOCK)[:, None]
    kj = jnp.arange(nk)[None, :]
    key_pos = jnp.arange(nb)[:, None, None] * C_BLOCK + kj[None] - C_BLOCK
    in_band = jnp.abs(qi + C_BLOCK - kj) <= C_WINDOW
    valid = in_band[None] & (key_pos >= 0) & (key_pos < n)
    s_band = jnp.einsum('bnqhgd,bnkhd->bnhgqk', qb, kb).astype(f32) * scale
    s_band = jnp.where(valid[None, :, None, None], s_band, NEG_INF)
    s_ctx = jnp.einsum('bnqhgd,bkhd->bnhgqk', qb, kc).astype(f32) * scale
    p = softmax_with_sink(jnp.concatenate([s_band, s_ctx], axis=-1), sink)
    o = (jnp.einsum('bnhgqk,bnkhd->bnqhgd', p[..., :nk].astype(vb.dtype), vb)
         + jnp.einsum('bnhgqk,bkhd->bnqhgd', p[..., nk:].astype(vc.dtype), vc))
    o_lat = o.reshape(b, n, C_HEADS * C_HEAD_DIM)
    o_ctx = None
    if with_ctx:
        s = jnp.einsum('bqhgd,bkhd->bhgqk', qc, kc).astype(f32) * scale
        pc = softmax_with_sink(s, sink).astype(vc.dtype)
        o_ctx = jnp.einsum('bhgqk,bkhd->bqhgd', pc, vc).reshape(b, qc.shape[1], C_HEADS * C_HEAD_DIM)
    return o_lat, o_ctx


def neighbourhood_attention(pd_lat, pd_ctx, rpb, with_ctx):
    def heads(p):
        b, n = p.shape[:2]
        return tuple(t.reshape(b, n, D_HEADS, D_HEAD_DIM) for t in jnp.split(p, 3, axis=-1))
    f32 = jnp.float32
    ql, kl, vl = heads(pd_lat)
    qc, kc, vc = heads(pd_ctx)
    b, n = pd_lat.shape[:2]
    rows = n // GRID_W
    kr = min(NA_ROWS, rows)
    n_nb = kr * NA_COLS
    scale = D_HEAD_DIM ** -0.5
    r_ids = jnp.arange(rows)
    row_start = jnp.clip(r_ids - kr // 2, 0, rows - kr)
    col = jnp.arange(GRID_W)
    col_idx = jnp.clip(col - NA_COLS // 2, 0, GRID_W - NA_COLS)[:, None] + jnp.arange(NA_COLS)[None]
    col_bias = col_idx - col[:, None] + NA_COLS - 1
    kg = kl.reshape(b, rows, GRID_W, D_HEADS, D_HEAD_DIM)
    vg = vl.reshape(b, rows, GRID_W, D_HEADS, D_HEAD_DIM)
    qg = jnp.moveaxis(ql.reshape(b, rows, GRID_W, D_HEADS, D_HEAD_DIM), 1, 0)
    rpb = rpb.astype(f32)

    def row_block(args):
        r_i, q_row = args
        rs = row_start[r_i]
        k_nb = lax.dynamic_slice_in_dim(kg, rs, kr, axis=1)[:, :, col_idx]
        v_nb = lax.dynamic_slice_in_dim(vg, rs, kr, axis=1)[:, :, col_idx]
        row_bias = rs + jnp.arange(kr) - r_i + NA_ROWS - 1
        bias = rpb[:, row_bias[:, None, None], col_bias[None]]
        s_nb = jnp.einsum('bchd,bicjhd->bhcij', q_row, k_nb).astype(f32) * scale + jnp.transpose(bias, (0, 2, 1, 3))[None]
        s_ctx = jnp.einsum('bchd,bkhd->bhck', q_row, kc).astype(f32) * scale
        p = jax.nn.softmax(jnp.concatenate([s_nb.reshape(b, D_HEADS, GRID_W, n_nb), s_ctx], axis=-1), axis=-1)
        p_nb = p[..., :n_nb].reshape(b, D_HEADS, GRID_W, kr, NA_COLS).astype(v_nb.dtype)
        return (jnp.einsum('bhcij,bicjhd->bchd', p_nb, v_nb)
                + jnp.einsum('bhck,bkhd->bchd', p[..., n_nb:].astype(vc.dtype), vc))

    o = lax.map(row_block, (r_ids, qg))
    o_lat = jnp.moveaxis(o, 0, 1).reshape(b, n, D_HEADS * D_HEAD_DIM)
    o_ctx = None
    if with_ctx:
        o_ctx = softmax_attend(qc, kc, vc, scale).reshape(b, qc.shape[1], D_HEADS * D_HEAD_DIM)
    return o_lat, o_ctx


def gated_merge(h, branches, w_branch, w_gate, b_gate, w_out):
    merged = jax.nn.sigmoid(h @ w_gate[0] + b_gate[0]) * (branches[0] @ w_branch[0])
    for i in range(1, N_BRANCHES):
        merged = merged + jax.nn.sigmoid(h @ w_gate[i] + b_gate[i]) * (branches[i] @ w_branch[i])
    return merged @ w_out


def peer(h, w_q, sub_keys, u_tab, v_tab):
    b, n, d = h.shape
    hb = h.reshape(b * n // PEER_BLOCK, PEER_BLOCK, d)

    def block(hx):
        q = (hx @ w_q).reshape(PEER_BLOCK, PEER_HEADS, 2, PEER_KEY_DIM // 2)
        s = jnp.einsum('thpd,hpnd->thpn', q, sub_keys).astype(jnp.float32)
        half_s, half_i = lax.top_k(s, PEER_TOPK)
        cand_s = (half_s[:, :, 0, :, None] + half_s[:, :, 1, None, :]).reshape(PEER_BLOCK, PEER_HEADS, PEER_TOPK ** 2)
        cand_i = (half_i[:, :, 0, :, None] * PEER_N_KEYS + half_i[:, :, 1, None, :]).reshape(PEER_BLOCK, PEER_HEADS, PEER_TOPK ** 2)
        top_s, pos = lax.top_k(cand_s, PEER_TOPK)
        experts = jnp.take_along_axis(cand_i, pos, axis=-1)
        g = jax.nn.softmax(top_s, axis=-1)
        act = jax.nn.gelu(jnp.einsum('td,thkd->thk', hx, u_tab[experts]).astype(jnp.float32), approximate=False)
        return jnp.einsum('thk,thkd->td', (g * act).astype(hx.dtype), v_tab[experts])

    return lax.map(block, hb).reshape(b, n, d)


def setup_inputs(seed: int = 0) -> dict:
    key = jax.random.key(seed)
    ks = jax.random.split(key, 32)
    f32 = jnp.float32
    D = D_MODEL

    def nrm(k, shape, scale):
        return scale * jax.random.normal(k, shape, f32)

    def gain(k, shape):
        return 1.0 + 0.01 * jax.random.normal(k, shape, f32)

    return {
        'x': nrm(ks[0], (BATCH, SEQ, D), 1.0),
        'c': nrm(ks[1], (BATCH, D), 1.0),
        'ctx': nrm(ks[2], (BATCH, CTX_LEN, D), 1.0),
        'c_ctx': nrm(ks[3], (D,), 1.0),
        'ada_w': nrm(ks[4], (DEPTH, D, 6 * D), 0.5 * D ** -0.5),
        'ada_b': nrm(ks[5], (DEPTH, 6 * D), 0.02),
        'norm_mix': gain(ks[6], (DEPTH, D)),
        'norm_ffn': gain(ks[7], (DEPTH, D)),
        'w_in': nrm(ks[8], (DEPTH, D, IN_WIDTH), D ** -0.5),
        'a_lam_q1': nrm(ks[9], (DEPTH, A_QK_DIM), 0.1),
        'a_lam_k1': nrm(ks[10], (DEPTH, A_QK_DIM), 0.1),
        'a_lam_q2': nrm(ks[11], (DEPTH, A_QK_DIM), 0.1),
        'a_lam_k2': nrm(ks[12], (DEPTH, A_QK_DIM), 0.1),
        'a_subln': gain(ks[13], (DEPTH, A_V_DIM)),
        'b_qa_norm': gain(ks[14], (DEPTH, B_Q_LORA)),
        'b_w_uq': nrm(ks[15], (DEPTH, B_Q_LORA, B_HEADS * (B_NOPE + B_ROPE)), B_Q_LORA ** -0.5),
        'b_kva_norm': gain(ks[16], (DEPTH, B_KV_LORA)),
        'b_w_ukv': nrm(ks[17], (DEPTH, B_KV_LORA, B_HEADS * (B_NOPE + B_V)), B_KV_LORA ** -0.5),
        'c_sink': nrm(ks[18], (DEPTH, C_HEADS), 0.5),
        'd_rpb': nrm(ks[19], (DEPTH, D_HEADS, 2 * NA_ROWS - 1, 2 * NA_COLS - 1), 0.05),
        'w_branch': nrm(ks[20], (DEPTH, N_BRANCHES, BRANCH_WIDTH, D), BRANCH_WIDTH ** -0.5),
        'w_gate': nrm(ks[21], (DEPTH, N_BRANCHES, D, D), D ** -0.5),
        'b_gate': nrm(ks[22], (DEPTH, N_BRANCHES, D), 0.02),
        'w_out': nrm(ks[23], (DEPTH, D, D), D ** -0.5),
        'peer_wq': nrm(ks[24], (DEPTH, D, PEER_HEADS * PEER_KEY_DIM), D ** -0.5),
        'peer_keys': nrm(ks[25], (DEPTH, PEER_HEADS, 2, PEER_N_KEYS, PEER_KEY_DIM // 2), (PEER_KEY_DIM // 2) ** -0.5),
        'peer_u': nrm(ks[26], (DEPTH, PEER_N_EXPERTS, D), D ** -0.5),
        'peer_v': nrm(ks[27], (DEPTH, PEER_N_EXPERTS, D), PEER_HEADS ** -0.5),
        'final_norm': gain(ks[28], (D,)),
    }


def reference(x, c, ctx, c_ctx, ada_w, ada_b, norm_mix, norm_ffn, w_in, a_lam_q1, a_lam_k1, a_lam_q2, a_lam_k2,
              a_subln, b_qa_norm, b_w_uq, b_kva_norm, b_w_ukv, c_sink, d_rpb, w_branch, w_gate, b_gate, w_out,
              peer_wq, peer_keys, peer_u, peer_v, final_norm):
    cos, sin = axial_rope(x.shape[1])
    xc = ctx
    for l in range(DEPTH):
        with_ctx = l < DEPTH - 1
        lam_init = 0.8 - 0.6 * math.exp(-0.3 * l)
        mod_lat = (jax.nn.silu(c) @ ada_w[l] + ada_b[l])[:, None, :]
        mod_ctx = (jax.nn.silu(c_ctx) @ ada_w[l] + ada_b[l])[None, None, :]
        sh1, sc1, g1, sh2, sc2, g2 = jnp.split(mod_lat, 6, axis=-1)
        sh1c, sc1c, g1c, sh2c, sc2c, g2c = jnp.split(mod_ctx, 6, axis=-1)

        h_lat = modulate(rms_norm(x, norm_mix[l]), sh1, sc1)
        h_ctx = modulate(rms_norm(xc, norm_mix[l]), sh1c, sc1c)
        pa, pb, pc, pd = jnp.split(h_lat @ w_in[l], IN_SPLITS, axis=-1)
        pac, pbc, pcc, pdc = jnp.split(h_ctx @ w_in[l], IN_SPLITS, axis=-1)
        oa, oac = diff_attention(pa, pac, cos, sin, a_lam_q1[l], a_lam_k1[l], a_lam_q2[l], a_lam_k2[l],
                                 a_subln[l], lam_init, with_ctx)
        ob, obc = latent_attention(pb, pbc, cos, sin, b_qa_norm[l], b_w_uq[l], b_kva_norm[l], b_w_ukv[l], with_ctx)
        oc, occ = window_attention(pc, pcc, cos, sin, c_sink[l], with_ctx)
        od, odc = neighbourhood_attention(pd, pdc, d_rpb[l], with_ctx)
        x = x + g1 * gated_merge(h_lat, (oa, ob, oc, od), w_branch[l], w_gate[l], b_gate[l], w_out[l])

        x = x + g2 * peer(modulate(rms_norm(x, norm_ffn[l]), sh2, sc2), peer_wq[l], peer_keys[l], peer_u[l], peer_v[l])

        if with_ctx:
            xc = xc + g1c * gated_merge(h_ctx, (oac, obc, occ, odc), w_branch[l], w_gate[l], b_gate[l], w_out[l])
            xc = xc + g2c * peer(modulate(rms_norm(xc, norm_ffn[l]), sh2c, sc2c), peer_wq[l], peer_keys[l], peer_u[l], peer_v[l])
    return rms_norm(x, final_norm)
```

```python
import math
import numpy as np
from contextlib import ExitStack
import concourse.bass as bass
import concourse.mybir as mybir
from concourse.bass_utils import run_bass_kernel_spmd

F32, BF16 = mybir.dt.float32, mybir.dt.bfloat16
AF = mybir.ActivationFunctionType
ALU = mybir.AluOpType
AX = mybir.AxisListType

D = 2048
KC = 16
CTX = 256
TB = 256
EPS = 1e-6
GRID_W = 64


class Dep:
    __slots__ = ("w", "r", "excl")

    def __init__(self):
        self.w = None
        self.r = []
        self.excl = False


class Prog:
    NS = 8
    COMPUTE = ("pe", "act", "dve", "pool")

    def __init__(self, nc, es):
        self.nc = nc
        self.sem = {}
        self.count = {}
        self.ops = {e: [] for e in ("pe", "act", "dve", "pool", "sp")}
        self.seen = {e: {} for e in self.ops}
        for e in self.COMPUTE:
            self.sem[e] = es.enter_context(nc.semaphore("s_" + e))
            self.count[e] = 0
        self.dsem = [es.enter_context(nc.semaphore("d%d" % i)) for i in range(self.NS)]
        self.dn = 0
        self.sb_off = 0
        self.sb_base = 0
        self.ARENA = 100 * 1024
        self.SB_BASE = 16512
        self.sb_n = 0
        self.banks = [es.enter_context(nc.psum_tensor("bank%d" % i, [128, 512], F32)) for i in range(8)]
        self.sdep = [Dep() for _ in range(8)]
        for d_ in self.sdep:
            d_.excl = True
        self.slo, self.shi, self.slot_i = 0, 8, 0

    def sb(self, shape, dtype):
        nbytes = int(np.prod(shape[1:])) * (4 if dtype == F32 else 2)
        nbytes = (nbytes + 63) // 64 * 64
        self.sb_n += 1
        h = self.nc.alloc_sbuf_tensor_at("t%d" % self.sb_n, list(shape), dtype, offset=self.SB_BASE + 2 * self.sb_off)
        self.sb_off += nbytes // 2
        assert self.sb_off <= self.ARENA, ("sbuf overflow", self.sb_off)
        if len(shape) == 2:
            return h[:, :]
        if len(shape) == 3:
            return h[:, :, :]
        return h[:, :, :, :]

    def phase(self):
        self.barrier()
        self.sb_off = self.sb_base
        self.set_slots(0, 8)

    def set_slots(self, lo=0, hi=8):
        self.slo, self.shi = lo, hi
        self.slot_i = lo

    def _next(self):
        b = self.slot_i
        self.slot_i = b + 1 if b + 1 < self.shi else self.slo
        return b

    def slot(self):
        b = self._next()
        return self.banks[b][:, 0:256], self.sdep[b]

    def bank(self):
        b = self._next()
        return self.banks[b][:, :], [self.sdep[b]]

    def bankn(self, b):
        return self.banks[b][:, :], [self.sdep[b]]

    def slotn(self, s):
        return self.banks[s // 2][:, (s % 2) * 256:(s % 2) * 256 + 256], self.sdep[s // 2]

    def _waits(self, eng, reads, writes):
        toks = []
        for d in reads:
            if d.w is not None:
                toks.append(d.w)
        for d in writes:
            if d.w is not None:
                toks.append(d.w)
            toks.extend(d.r)
        need = {}
        for (key, sem, val) in toks:
            if key == "pe" and eng == "pe":
                continue
            if self.seen[eng].get(key, 0) >= val:
                continue
            if need.get(key, (None, 0))[1] < val:
                need[key] = (sem, val)
        for key, (sem, val) in need.items():
            self.seen[eng][key] = val
        return list(need.values())

    def _mark(self, tok, reads, writes):
        for d in reads:
            d.r.append(tok)
        for d in writes:
            d.w = tok
            d.r = []

    def op(self, eng, fn, reads=(), writes=()):
        if any(d.excl for d in reads):
            writes = list(writes) + [d for d in reads if d.excl]
            reads = [d for d in reads if not d.excl]
        waits = self._waits(eng, reads, writes)
        self.count[eng] += 1
        tok = (eng, self.sem[eng], self.count[eng])
        self.ops[eng].append((waits, fn, (self.sem[eng], 1)))
        self._mark(tok, reads, writes)
        return tok

    def dma(self, out, in_, reads=(), writes=()):
        eng = "sp"
        n = self.dn
        self.dn += 1
        slot = n % self.NS
        key = "dma%d" % slot
        waits = self._waits(eng, reads, writes)
        prev = 16 * (n // self.NS)
        if prev > 0 and self.seen[eng].get(key, 0) < prev:
            waits.append((self.dsem[slot], prev))
            self.seen[eng][key] = prev
        tok = (key, self.dsem[slot], prev + 16)
        self.ops[eng].append((waits, lambda e, o=out, i=in_: e.dma_start(out=o, in_=i), (self.dsem[slot], 16)))
        self._mark(tok, reads, writes)
        return tok

    def barrier(self):
        toks = [(e, self.sem[e], self.count[e]) for e in self.COMPUTE if self.count[e] > 0]
        for s in range(self.NS):
            cnt = (self.dn - s + self.NS - 1) // self.NS
            if cnt > 0:
                toks.append(("dma%d" % s, self.dsem[s], 16 * cnt))
        for eng in self.ops:
            waits = []
            for (key, sem, val) in toks:
                if self.seen[eng].get(key, 0) < val:
                    waits.append((sem, val))
                    self.seen[eng][key] = val
            if waits:
                self.ops[eng].append((waits, None, None))

    def emit(self):
        handles = {"pe": "tensor", "act": "scalar", "dve": "vector", "pool": "gpsimd", "sp": "sync"}
        with self.nc.Block() as block:
            for eng, attr in handles.items():
                ops = self.ops[eng]

                def body(e, ops=ops):
                    for (waits, fn, inc) in ops:
                        for (sem, val) in waits:
                            e.wait_ge(sem, val)
                        if fn is not None:
                            ins = fn(e)
                            if inc is not None:
                                ins.then_inc(inc[0], inc[1])
                getattr(block, attr)(body)


SECTIONS = [
    ("qA", 512, "rope"), ("kA", 512, "rope"), ("qC", 512, "rope"), ("kC2", 256, "rope"), ("kpe2", 128, "rope"),
    ("qD", 512, "fm"), ("kD", 512, "fm"), ("pq", 512, "lora"), ("ckv", 256, "lora"),
    ("vA", 512, "tm"), ("vD", 512, "tm"), ("vC", 128, "tm"),
]


def section_offsets():
    off = 0
    res = {}
    for (nm, w, kind) in SECTIONS:
        res[nm] = off
        off += w * (2 if kind == "rope" else 1)
    return res, off


SEC_OFF, NPROJ = section_offsets()

A_IN, B_IN, C_IN = 1536, 832, 768
_A0, _B0, _C0, _D0 = 0, 1536, 1536 + 832, 1536 + 832 + 768


def _swap64(w):
    k, n = w.shape
    return w.reshape(k, n // 64, 2, 32)[:, :, ::-1, :].reshape(k, n)


def build_wproj(w_in_l):
    a = w_in_l[:, _A0:_A0 + A_IN]
    b = w_in_l[:, _B0:_B0 + B_IN]
    c = w_in_l[:, _C0:_C0 + C_IN]
    d = w_in_l[:, _D0:]
    qA, kA, vA = a[:, 0:512], a[:, 512:1024], a[:, 1024:1536]
    pq, ckv, kpe = b[:, 0:512], b[:, 512:768], b[:, 768:832]
    qC, kC, vC = c[:, 0:512], c[:, 512:640], c[:, 640:768]
    qD, kD, vD = d[:, 0:512], d[:, 512:1024], d[:, 1024:1536]
    kC2 = np.concatenate([kC[:, 0:64], kC[:, 0:64], kC[:, 64:128], kC[:, 64:128]], axis=1)
    kpe2 = np.concatenate([kpe, kpe], axis=1)
    parts = {"qA": qA, "kA": kA, "qC": qC, "kC2": kC2, "kpe2": kpe2, "qD": qD, "kD": kD, "pq": pq, "ckv": ckv,
             "vA": vA, "vD": vD, "vC": vC}
    cols = []
    for (nm, w, kind) in SECTIONS:
        cols.append(parts[nm])
        if kind == "rope":
            cols.append(_swap64(parts[nm]))
    out = np.concatenate(cols, axis=1)
    assert out.shape[1] == NPROJ
    return np.ascontiguousarray(out)


def rope_tables(SEQ):
    t = np.arange(SEQ)
    row = (t // GRID_W).astype(np.float32)
    col = (t % GRID_W).astype(np.float32)
    inv = (10000.0 ** (-np.arange(16, dtype=np.float32) / 16)).astype(np.float32)
    ang = np.concatenate([row[:, None] * inv, col[:, None] * inv], axis=-1)
    cos, sin = np.cos(ang), np.sin(ang)
    T = SEQ + CTX
    cs = np.ones((128, T), np.float32)
    sn = np.zeros((128, T), np.float32)
    for p in range(128):
        i = p % 32
        cs[p, :SEQ] = cos[:, i]
        sn[p, :SEQ] = sin[:, i] * (-1.0 if (p % 64) < 32 else 1.0)
    return cs, sn


def pcl(v, nch):
    return np.ascontiguousarray(v.reshape(nch, 128).T)


class Net:
    def __init__(self, SEQ, DEPTH, dbg=()):
        self.SEQ, self.L = SEQ, DEPTH
        self.T = SEQ + CTX
        self.NB = self.T // TB
        self.NKT = self.T // 128
        self.NLT = SEQ // 128
        self.dbg = set(dbg)
        self.nc = bass.Bass("TRN2", target_bir_lowering=False)
        self.i = {}
        self.s = {}
        self.dd = {}
        self.out_names = []

    def din(self, name, shape, dt=F32):
        self.i[name] = self.nc.dram_tensor(name, list(shape), dt, kind="ExternalInput").ap()

    def dscr(self, name, shape, dt=BF16):
        if name in self.dbg:
            self.s[name] = self.nc.dram_tensor(name, list(shape), dt, kind="ExternalOutput").ap()
            self.out_names.append(name)
        else:
            self.s[name] = self.nc.dram_tensor(name, list(shape), dt).ap()

    def dep(self, *key):
        d = self.dd.get(key)
        if d is None:
            d = self.dd[key] = Dep()
        return d

    def bdeps(self, name):
        return [self.dep(name, b) for b in range(self.NB)]

    def declare(self):
        L, T, SEQ = self.L, self.T, self.SEQ
        di = self.din
        di("xT0", [D, T]); di("cc", [128, 32]); di("ada_w", [L, D, 6 * D]); di("adab", [L, 128, 96])
        di("nmix", [L, 128, 16]); di("nffn", [L, 128, 16]); di("wproj", [L, D, NPROJ])
        di("lamp", [L, 1, 256]); di("subln", [L, 1, 128]); di("qan", [L, 128, 4]); di("wuq2", [L, 512, 1024])
        di("kvn", [L, 128, 2]); di("wukv2", [L, 256, 1024]); di("sink", [L, 1, 8]); di("rpbr", [L, 60, 31])
        di("wbr", [L, D, D]); di("wg", [L, 4 * D, D]); di("bg", [L, 128, 64]); di("wout", [L, D, D])
        di("wq", [L, D, D]); di("keysT", [L, 128, 2048]); di("uT", [L, D, 16384]); di("v", [L, 16384, D])
        di("lamc", [1, 2]); di("fnorm", [128, 16]); di("cs", [128, T]); di("sn", [128, T]); di("cmask", [128, 256]); di("colm", [128, 64])
        self.yT = self.nc.dram_tensor("yT", [D, SEQ], F32, kind="ExternalOutput").ap()
        ds = self.dscr
        ds("xT", [D, T], F32); ds("hT", [D, T])
        ds("wprojb", [D, NPROJ]); ds("wuqb", [512, 1024]); ds("wukvb", [256, 1024])
        ds("wprojt", [128, NPROJ // 128, 16, 128]); ds("wgt", [128, 64, 16, 128]); ds("wbt", [128, 64, 4, 128])
        ds("wot", [128, 16, 16, 128]); ds("wqt", [128, 16, 16, 128]); ds("uTt", [128, 128, 16, 128])
        ds("vt", [128, 32, 2, 4, 1024])
        ds("qA", [512, T]); ds("kA", [512, T]); ds("vA", [T, 512])
        ds("qBn", [512, T]); ds("qBpe", [256, T]); ds("kBn", [512, T]); ds("kpe2", [128, T]); ds("vB", [T, 512])
        ds("qC", [512, T]); ds("kC2", [256, T]); ds("vC", [T, 128])
        ds("qD", [512, T]); ds("kD", [512, T]); ds("vD", [T, 512])
        ds("oT", [D, T]); ds("EP", [60, 160], F32)

    def persist(self, P):
        self.ident = P.sb([128, 128], BF16)
        self.identf = P.sb([128, 128], F32)
        self.onesf = P.sb([128, 128], F32)
        self.csil = P.sb([128, 32], F32)
        self.mod = P.sb([128, 96, 2], F32)
        self.a1 = P.sb([128, 16, 2], F32)
        self.a2 = P.sb([128, 16, 2], F32)
        self.nm = P.sb([128, 32], F32)
        self.adab = P.sb([128, 96], F32)
        self.pd = Dep()
        P.sb_base = P.sb_off
        ident, identf, onesf, csil = self.ident, self.identf, self.onesf, self.csil
        P.op("pool", lambda e: e.memset(identf, 0.0), writes=[self.pd])
        P.op("pool", lambda e: e.affine_select(out=identf, in_=identf, pattern=[[-1, 128]], compare_op=ALU.not_equal,
                                               fill=1.0, base=0, channel_multiplier=1), reads=[self.pd], writes=[self.pd])
        P.op("dve", lambda e: e.tensor_copy(out=ident, in_=identf), reads=[self.pd], writes=[self.pd])
        P.op("pool", lambda e: e.memset(onesf, 1.0), writes=[self.pd])
        P.dma(csil, self.i["cc"][:, :], writes=[self.pd])
        P.op("act", lambda e: e.activation(out=csil, in_=csil, func=AF.Silu), reads=[self.pd], writes=[self.pd])

    def cast2d(self, P, src, R, C, ddep, dstf):
        CW = 2048
        jobs = [(r0, c0, min(CW, C - c0)) for r0 in range(0, R, 128) for c0 in range(0, C, CW)]
        bufs = self.cast_bufs

        def load(k):
            r0, c0, w = jobs[k]
            st, so, ds_, do = bufs[(self.cast_i + k) % 3]
            P.dma(st[:, 0:w], src[r0:r0 + 128, c0:c0 + w], writes=[ds_])

        for k in range(min(2, len(jobs))):
            load(k)
        for k, (r0, c0, w) in enumerate(jobs):
            st, so, ds_, do = bufs[(self.cast_i + k) % 3]
            eng = ("act", "dve", "pool")[(self.cast_i + k) % 3]
            if eng == "act":
                P.op("act", lambda e, so=so, st=st, w=w: e.copy(out=so[:, 0:w], in_=st[:, 0:w]), reads=[ds_], writes=[do])
            else:
                P.op(eng, lambda e, so=so, st=st, w=w: e.tensor_copy(out=so[:, 0:w], in_=st[:, 0:w]), reads=[ds_], writes=[do])
            if k + 2 < len(jobs):
                load(k + 2)
            for (dap, sview) in dstf(r0, c0, w, so):
                P.dma(dap, sview, reads=[do], writes=[ddep])
        self.cast_i += len(jobs)

    def cast_phase(self, P, l):
        P.phase()
        self.cast_bufs = [(P.sb([128, 2048], F32), P.sb([128, 2048], BF16), Dep(), Dep()) for _ in range(3)]
        self.cast_i = 0
        i, s = self.i, self.s

        def nat(name):
            return lambda r0, c0, w, so: [(s[name][r0:r0 + 128, c0:c0 + w], so[:, 0:w])]

        def chunked(name, extra=None):
            def f(r0, c0, w, so):
                res = [(s[name][:, c0 // 128:(c0 + w) // 128, r0 // 128, :], so[:, 0:w].rearrange("p (c n) -> p c n", n=128))]
                if extra is not None:
                    res += extra(r0, c0, w, so)
                return res
            return f

        def gate_dst(r0, c0, w, so):
            br, kc = r0 // D, (r0 % D) // 128
            return [(s["wgt"][:, br * 16 + c0 // 128:br * 16 + (c0 + w) // 128, kc, :], so[:, 0:w].rearrange("p (c n) -> p c n", n=128))]

        def br_dst(r0, c0, w, so):
            br, kc = r0 // 512, (r0 % 512) // 128
            return [(s["wbt"][:, br * 16 + c0 // 128:br * 16 + (c0 + w) // 128, kc, :], so[:, 0:w].rearrange("p (c n) -> p c n", n=128))]

        def v_dst(r0, c0, w, so):
            eg, c = r0 // 512, (r0 % 512) // 128
            return [(s["vt"][:, eg, dh, c, :], so[:, dh * 1024:(dh + 1) * 1024]) for dh in range(2)]

        self.cast2d(P, i["wproj"][l], D, NPROJ, self.dep("wprojb"), chunked("wprojt", nat("wprojb")))
        self.cast2d(P, i["wuq2"][l], 512, 1024, self.dep("wuqb"), nat("wuqb"))
        self.cast2d(P, i["wukv2"][l], 256, 1024, self.dep("wukvb"), nat("wukvb"))
        self.cast2d(P, i["wg"][l], 4 * D, D, self.dep("wgb"), gate_dst)
        self.cast2d(P, i["wbr"][l], D, D, self.dep("wbb"), br_dst)
        self.cast2d(P, i["wout"][l], D, D, self.dep("wob"), chunked("wot"))
        self.cast2d(P, i["wq"][l], D, D, self.dep("wqb"), chunked("wqt"))
        self.cast2d(P, i["uT"][l], D, 16384, self.dep("uTb"), chunked("uTt"))
        self.cast2d(P, i["v"][l], 16384, D, self.dep("vb"), v_dst)

    def adaln_phase(self, P, l):
        P.phase()
        i = self.i
        mod, csil, adab, nm, pd = self.mod, self.csil, self.adab, self.nm, self.pd
        P.dma(adab, i["adab"][l], writes=[pd])
        P.dma(nm[:, 0:16], i["nmix"][l], writes=[pd])
        P.dma(nm[:, 16:32], i["nffn"][l], writes=[pd])
        wt = [(P.sb([128, 16, 512], F32), Dep()) for _ in range(2)]
        src = i["ada_w"][l].rearrange("(c p) n -> p c n", p=128)
        NG = 6 * D // 512
        P.dma(wt[0][0], src[:, :, 0:512], writes=[wt[0][1]])
        for g in range(NG):
            w, wd = wt[g % 2]
            if g + 1 < NG:
                w2, wd2 = wt[(g + 1) % 2]
                P.dma(w2, src[:, :, (g + 1) * 512:(g + 2) * 512], writes=[wd2])
            for j in range(4):
                ch = g * 4 + j
                ps, psd = P.slot()
                for kc in range(KC):
                    P.op("pe", lambda e, ps=ps, w=w, kc=kc, j=j: e.matmul(ps[:, 0:2], lhsT=w[:, kc, j * 128:(j + 1) * 128],
                                                                          rhs=csil[:, kc * 2:kc * 2 + 2], start=(kc == 0), stop=(kc == KC - 1)),
                         reads=[wd, pd], writes=[psd])
                P.op("dve", lambda e, ps=ps, ch=ch: e.tensor_scalar(out=mod[:, ch, :], in0=ps[:, 0:2], scalar1=adab[:, ch:ch + 1], scalar2=None,
                                                                    op0=ALU.add), reads=[psd, pd], writes=[pd])
        a1, a2 = self.a1, self.a2
        P.op("dve", lambda e: e.tensor_scalar(out=a1, in0=mod[:, 16:32, :], scalar1=1.0, scalar2=None, op0=ALU.add), reads=[pd], writes=[pd])
        P.op("dve", lambda e: e.tensor_tensor(out=a1, in0=a1, in1=nm[:, 0:16, None].broadcast_to([128, 16, 2]), op=ALU.mult), reads=[pd], writes=[pd])
        P.op("dve", lambda e: e.tensor_scalar(out=a2, in0=mod[:, 64:80, :], scalar1=1.0, scalar2=None, op0=ALU.add), reads=[pd], writes=[pd])
        P.op("dve", lambda e: e.tensor_tensor(out=a2, in0=a2, in1=nm[:, 16:32, None].broadcast_to([128, 16, 2]), op=ALU.mult), reads=[pd], writes=[pd])

    def norm_phase(self, P, src, srcname, a_ap, b_ap, dst, dstname, out_f32=False, nblocks=None):
        P.phase()
        pd = self.pd
        NBK = self.NB if nblocks is None else nblocks
        xts = [(P.sb([128, 16, 256], F32), Dep()) for _ in range(2)]
        sqs = [(P.sb([128, 256], F32), Dep()) for _ in range(4)]
        tmps = [(P.sb([128, 256], F32), Dep()) for _ in range(4)]
        rstd = P.sb([128, 256], F32)
        rd = Dep()
        odt = F32 if out_f32 else BF16
        hbs = [(P.sb([128, 16, 256], odt), Dep()) for _ in range(2)]
        srcv = src.rearrange("(c p) t -> p c t", p=128)
        dstv = dst.rearrange("(c p) t -> p c t", p=128)
        P.dma(xts[0][0], srcv[:, :, 0:TB], reads=[self.dep(srcname, 0)], writes=[xts[0][1]])
        k = 0
        for tb in range(NBK):
            xt, xd = xts[tb % 2]
            hb, hd = hbs[tb % 2]
            j = 1 if tb == self.NB - 1 else 0
            if tb + 1 < NBK:
                P.dma(xts[(tb + 1) % 2][0], srcv[:, :, (tb + 1) * TB:(tb + 2) * TB], reads=[self.dep(srcname, tb + 1)],
                      writes=[xts[(tb + 1) % 2][1]])
            ps, psd = P.slot()
            for c in range(KC):
                sq, sqd = sqs[k % 4]
                k += 1
                P.op("act", lambda e, sq=sq, xt=xt, c=c: e.activation(out=sq, in_=xt[:, c, :], func=AF.Square), reads=[xd], writes=[sqd])
                P.op("pe", lambda e, ps=ps, sq=sq, c=c: e.matmul(ps, lhsT=self.onesf, rhs=sq, start=(c == 0), stop=(c == KC - 1)),
                     reads=[sqd, pd], writes=[psd])
            P.op("act", lambda e, ps=ps: e.activation(out=rstd, in_=ps, func=AF.Sqrt, bias=EPS, scale=1.0 / D), reads=[psd], writes=[rd])
            P.op("dve", lambda e: e.reciprocal(out=rstd, in_=rstd), reads=[rd], writes=[rd])
            for c in range(KC):
                tmp, td = tmps[k % 4]
                k += 1
                P.op("dve", lambda e, tmp=tmp, xt=xt, c=c, j=j: e.scalar_tensor_tensor(out=tmp, in0=xt[:, c, :], scalar=a_ap[:, c, j:j + 1], in1=rstd,
                                                                                  op0=ALU.mult, op1=ALU.mult), reads=[xd, rd, pd], writes=[td])
                if b_ap is not None:
                    P.op("act", lambda e, hb=hb, tmp=tmp, c=c, j=j: e.activation(out=hb[:, c, :], in_=tmp, func=AF.Identity, bias=b_ap[:, c, j:j + 1]),
                         reads=[td, pd], writes=[hd])
                else:
                    P.op("act", lambda e, hb=hb, tmp=tmp, c=c: e.copy(out=hb[:, c, :], in_=tmp), reads=[td], writes=[hd])
            P.dma(dstv[:, :, tb * TB:(tb + 1) * TB], hb, reads=[hd], writes=[self.dep(dstname, tb)])

    def mm_chain(self, P, out, outdeps, pairs, reads):
        n = len(pairs)
        for k, (lh, rh) in enumerate(pairs):
            P.op("pe", lambda e, lh=lh, rh=rh, k=k: e.matmul(out, lhsT=lh, rhs=rh, start=(k == 0), stop=(k == n - 1)),
                 reads=reads, writes=outdeps)

    def proj_phase(self, P, l):
        P.phase()
        s, i, pd = self.s, self.i, self.pd
        NB = self.NB
        hbs = [(P.sb([128, 16, 256], BF16), Dep()) for _ in range(2)]
        css = [(P.sb([128, 256], F32), P.sb([128, 256], F32), Dep()) for _ in range(2)]
        wts = [(P.sb([128, 16, 512], BF16), Dep()) for _ in range(3)]
        wtc = [(w_.rearrange("p k n -> p (k n)").rearrange("p (c k n) -> p c k n", c=4, k=16), d_) for (w_, d_) in wts]
        wprojt = s["wprojt"]
        wuq = P.sb([128, 4, 1024], BF16)
        wukv = P.sb([128, 2, 1024], BF16)
        nrm = P.sb([128, 8], F32)
        wsd = Dep()
        P.dma(wuq, s["wuqb"].rearrange("(c p) n -> p c n", p=128), reads=[self.dep("wuqb")], writes=[wsd])
        P.dma(wukv, s["wukvb"].rearrange("(c p) n -> p c n", p=128), reads=[self.dep("wukvb")], writes=[wsd])
        P.dma(nrm[:, 0:4], i["qan"][l], writes=[wsd])
        P.dma(nrm[:, 4:6], i["kvn"][l], writes=[wsd])
        stages = [(P.sb([128, 4, 256], BF16), Dep()) for _ in range(3)]
        t1s = [(P.sb([128, 256], F32), Dep()) for _ in range(4)]
        vsts = [(P.sb([128, 512], BF16), Dep()) for _ in range(2)]
        pqf = P.sb([128, 4, 256], F32)
        ckvf = P.sb([128, 2, 256], F32)
        pqn = P.sb([128, 4, 256], BF16)
        ckvn = P.sb([128, 2, 256], BF16)
        lod = Dep()
        sqs = [(P.sb([128, 256], F32), Dep()) for _ in range(2)]
        rstd = P.sb([128, 256], F32)
        rd = Dep()
        wprojv = s["wprojb"].rearrange("(c p) n -> p c n", p=128)
        hTv = s["hT"].rearrange("(c p) t -> p c t", p=128)
        cnt = {"w": 0, "st": 0, "t": 0, "v": 0, "sq": 0}

        def load_w(c0, w, c1=None, w1=0):
            wt, wd = wts[cnt["w"] % 3]
            cnt["w"] += 1
            P.dma(wt[:, :, 0:w], wprojv[:, :, c0:c0 + w], reads=[self.dep("wprojb")], writes=[wd])
            if c1 is not None:
                P.dma(wt[:, :, w:w + w1], wprojv[:, :, c1:c1 + w1], reads=[self.dep("wprojb")], writes=[wd])
            return wt, wd

        def load_wc(c0, w, c1=None, w1=0):
            wt, wd = wtc[cnt["w"] % 3]
            cnt["w"] += 1
            n0 = w // 128
            P.dma(wt[:, 0:n0, :, :], wprojt[:, c0 // 128:c0 // 128 + n0, :, :], reads=[self.dep("wprojb")], writes=[wd])
            if c1 is not None:
                n1 = w1 // 128
                P.dma(wt[:, n0:n0 + n1, :, :], wprojt[:, c1 // 128:c1 // 128 + n1, :, :], reads=[self.dep("wprojb")], writes=[wd])
            return wt, wd

        def rope_chunk(psA, pdA, psB, pdB, cs_t, sn_t, csd, dst_ap, dstdep):
            ta, tad = t1s[cnt["t"] % 4]
            tb_, tbd = t1s[(cnt["t"] + 1) % 4]
            cnt["t"] += 2
            P.op("dve", lambda e: e.tensor_tensor(out=ta, in0=psA, in1=cs_t, op=ALU.mult), reads=[pdA, csd], writes=[tad])
            P.op("dve", lambda e: e.tensor_tensor(out=tb_, in0=psB, in1=sn_t, op=ALU.mult), reads=[pdB, csd], writes=[tbd])
            P.op("dve", lambda e: e.tensor_tensor(out=dst_ap, in0=ta, in1=tb_, op=ALU.add), reads=[tad, tbd], writes=[dstdep])

        def store_fm(name, stage, sd, nch, tb, row0=0):
            dst = s[name][row0:row0 + nch * 128, tb * TB:(tb + 1) * TB].rearrange("(c p) t -> p c t", p=128)
            P.dma(dst, stage[:, 0:nch, :], reads=[sd], writes=[self.dep(name, tb)])

        import os
        LIM = int(os.environ.get("PROJ_LIMIT", "99"))
        for tb in range(NB if LIM >= 99 else 1):
            hb, hd = hbs[tb % 2]
            cs_t, sn_t, csd = css[tb % 2]
            P.dma(hb, hTv[:, :, tb * TB:(tb + 1) * TB], reads=[self.dep("hT", tb)], writes=[hd])
            P.dma(cs_t, i["cs"][:, tb * TB:(tb + 1) * TB], writes=[csd])
            P.dma(sn_t, i["sn"][:, tb * TB:(tb + 1) * TB], writes=[csd])
            for si_, (nm, w, kind) in enumerate(SECTIONS):
                if si_ >= LIM:
                    break
                off = SEC_OFF[nm]
                if kind == "rope":
                    gw = min(256, w)
                    for g0 in range(0, w, gw):
                        wt, wd = load_wc(off + g0, gw, off + w + g0, gw)
                        stage, sd = stages[cnt["st"] % 3]
                        cnt["st"] += 1
                        SUB = int(os.environ.get("PROJ_SUB", "9"))
                        for j in range(gw // 128):
                            if SUB < 2:
                                continue
                            psA, pdA = P.slot()
                            psB, pdB = P.slot()
                            self.mm_chain(P, psA, [pdA], [(wt[:, j, kc, :], hb[:, kc, :]) for kc in range(KC)], [wd, hd])
                            self.mm_chain(P, psB, [pdB], [(wt[:, gw // 128 + j, kc, :], hb[:, kc, :]) for kc in range(KC)], [wd, hd])
                            if SUB >= 3:
                                rope_chunk(psA, pdA, psB, pdB, cs_t, sn_t, csd, stage[:, j, :], sd)
                        if SUB >= 4:
                            store_fm(nm, stage, sd, gw // 128, tb, row0=g0)
                elif kind in ("fm", "lora"):
                    wt, wd = load_wc(off, w)
                    if kind == "fm":
                        stage, sd = stages[cnt["st"] % 3]
                        cnt["st"] += 1
                    for j in range(w // 128):
                        ps, psd = P.slot()
                        self.mm_chain(P, ps, [psd], [(wt[:, j, kc, :], hb[:, kc, :]) for kc in range(KC)], [wd, hd])
                        if kind == "fm":
                            P.op("act", lambda e, stage=stage, j=j, ps=ps: e.copy(out=stage[:, j, :], in_=ps), reads=[psd], writes=[sd])
                        else:
                            dstt = pqf if nm == "pq" else ckvf
                            P.op("act", lambda e, dstt=dstt, j=j, ps=ps: e.copy(out=dstt[:, j, :], in_=ps), reads=[psd], writes=[lod])
                    if kind == "fm":
                        store_fm(nm, stage, sd, w // 128, tb)
                else:
                    wt, wd = load_w(off, w)
                    for tt in range(2):
                        bk, bds = P.bank()
                        self.mm_chain(P, bk[:, 0:w], bds, [(hb[:, kc, tt * 128:(tt + 1) * 128], wt[:, kc, 0:w]) for kc in range(KC)], [wd, hd])
                        vst, vd = vsts[cnt["v"] % 2]
                        cnt["v"] += 1
                        P.op("act", lambda e, vst=vst, bk=bk, w=w: e.copy(out=vst[:, 0:w], in_=bk[:, 0:w]), reads=bds, writes=[vd])
                        r0 = tb * TB + tt * 128
                        P.dma(s[nm][r0:r0 + 128, 0:w], vst[:, 0:w], reads=[vd], writes=[self.dep(nm, tb)])
            if LIM < 99:
                continue
            for (src, dstn, nch, g0, dim) in ((pqf, pqn, 4, 0, 512), (ckvf, ckvn, 2, 4, 256)):
                ps, psd = P.slot()
                for c in range(nch):
                    sq, sqd = sqs[cnt["sq"] % 2]
                    cnt["sq"] += 1
                    P.op("act", lambda e, sq=sq, src=src, c=c: e.activation(out=sq, in_=src[:, c, :], func=AF.Square), reads=[lod], writes=[sqd])
                    P.op("pe", lambda e, ps=ps, sq=sq, c=c, nch=nch: e.matmul(ps, lhsT=self.onesf, rhs=sq, start=(c == 0), stop=(c == nch - 1)),
                         reads=[sqd, pd], writes=[psd])
                P.op("act", lambda e, ps=ps, dim=dim: e.activation(out=rstd, in_=ps, func=AF.Sqrt, bias=EPS, scale=1.0 / dim), reads=[psd], writes=[rd])
                P.op("dve", lambda e: e.reciprocal(out=rstd, in_=rstd), reads=[rd], writes=[rd])
                for c in range(nch):
                    P.op("dve", lambda e, dstn=dstn, src=src, c=c, g0=g0: e.scalar_tensor_tensor(out=dstn[:, c, :], in0=src[:, c, :],
                                                                                           scalar=nrm[:, g0 + c:g0 + c + 1], in1=rstd,
                                                                                           op0=ALU.mult, op1=ALU.mult),
                         reads=[lod, rd, wsd], writes=[lod])
            stage, sd = stages[cnt["st"] % 3]
            cnt["st"] += 1
            for h in range(4):
                ps, psd = P.slot()
                self.mm_chain(P, ps, [psd], [(wuq[:, kc, h * 128:(h + 1) * 128], pqn[:, kc, :]) for kc in range(4)], [wsd, lod])
                P.op("act", lambda e, stage=stage, h=h, ps=ps: e.copy(out=stage[:, h, :], in_=ps), reads=[psd], writes=[sd])
            store_fm("qBn", stage, sd, 4, tb)
            stage, sd = stages[cnt["st"] % 3]
            cnt["st"] += 1
            for j in range(2):
                psA, pdA = P.slot()
                psB, pdB = P.slot()
                self.mm_chain(P, psA, [pdA], [(wuq[:, kc, 512 + j * 128:512 + (j + 1) * 128], pqn[:, kc, :]) for kc in range(4)], [wsd, lod])
                self.mm_chain(P, psB, [pdB], [(wuq[:, kc, 768 + j * 128:768 + (j + 1) * 128], pqn[:, kc, :]) for kc in range(4)], [wsd, lod])
                rope_chunk(psA, pdA, psB, pdB, cs_t, sn_t, csd, stage[:, j, :], sd)
            store_fm("qBpe", stage, sd, 2, tb)
            stage, sd = stages[cnt["st"] % 3]
            cnt["st"] += 1
            for h in range(4):
                ps, psd = P.slot()
                self.mm_chain(P, ps, [psd], [(wukv[:, kc, h * 128:(h + 1) * 128], ckvn[:, kc, :]) for kc in range(2)], [wsd, lod])
                P.op("act", lambda e, stage=stage, h=h, ps=ps: e.copy(out=stage[:, h, :], in_=ps), reads=[psd], writes=[sd])
            store_fm("kBn", stage, sd, 4, tb)
            for tt in range(2):
                bk, bds = P.bank()
                self.mm_chain(P, bk, bds, [(ckvn[:, kc, tt * 128:(tt + 1) * 128], wukv[:, kc, 512:1024]) for kc in range(2)], [wsd, lod])
                vst, vd = vsts[cnt["v"] % 2]
                cnt["v"] += 1
                P.op("act", lambda e, vst=vst, bk=bk: e.copy(out=vst, in_=bk), reads=bds, writes=[vd])
                r0 = tb * TB + tt * 128
                P.dma(s["vB"][r0:r0 + 128, :], vst, reads=[vd], writes=[self.dep("vB", tb)])

    def load_vaug(self, P, Vaug, vd, name, nh, dv):
        P.op("pool", lambda e: e.memset(Vaug, 1.0), writes=[vd])
        src = self.s[name]
        for kt in range(self.NKT):
            P.dma(Vaug[:, kt, :, 0:dv], src[kt * 128:(kt + 1) * 128, :].rearrange("p (h d) -> p h d", h=nh),
                  reads=[self.dep(name, kt // 2)], writes=[vd])

    def transpose_out(self, P, onb, ond, nch, OT, otd, qs):
        for c0 in range(0, nch, 4):
            n = min(4, nch - c0)
            ps, psd = P.slot()
            psb = ps.bitcast(BF16)
            for c in range(n):
                P.op("pe", lambda e, psb=psb, c=c, c0=c0: e.transpose(psb[:, c * 128:(c + 1) * 128], onb[:, (c0 + c) * 128:(c0 + c + 1) * 128], self.ident),
                     reads=[ond, self.pd], writes=[psd])
            P.op("act", lambda e, psb=psb, n=n, c0=c0: e.copy(out=OT[:, c0:c0 + n, qs * 128:(qs + 1) * 128],
                                                              in_=psb[:, 0:n * 128].rearrange("p (c q) -> p c q", c=n)),
                 reads=[psd], writes=[otd])

    def attnA_phase(self, P, l):
        P.phase()
        s, i, pd = self.s, self.i, self.pd
        T, NKT, NLT, NB = self.T, self.NKT, self.NLT, self.NB
        lam_init = 0.8 - 0.6 * math.exp(-0.3 * l)
        KT = P.sb([128, 4, T], BF16)
        kd = Dep()
        Vaug = P.sb([128, NKT, 4, 130], BF16)
        vd = Dep()
        P.dma(KT, s["kA"].rearrange("(c p) t -> p c t", p=128), reads=self.bdeps("kA"), writes=[kd])
        self.load_vaug(P, Vaug, vd, "vA", 4, 128)
        lp = P.sb([128, 256], F32)
        sm = P.sb([128, 8], F32)
        gs = P.sb([128, 128], F32)
        ld = Dep()
        lc = P.sb([128, 2], F32)
        P.dma(lc, i["lamc"][0:1, :].broadcast_to([128, 2]), writes=[ld])
        P.dma(lp, i["lamp"][l][0:1, :].broadcast_to([128, 256]), writes=[ld])
        P.dma(gs, i["subln"][l][0:1, :].broadcast_to([128, 128]), writes=[ld])
        P.op("dve", lambda e: e.tensor_tensor(out=lp[:, 0:128], in0=lp[:, 0:128], in1=lp[:, 128:256], op=ALU.mult), reads=[ld], writes=[ld])
        P.op("dve", lambda e: e.tensor_reduce(out=sm[:, 0:2], in_=lp[:, 0:128].rearrange("p (a b) -> p a b", a=2), axis=AX.X, op=ALU.add),
             reads=[ld], writes=[ld])
        P.op("act", lambda e: e.activation(out=sm[:, 0:2], in_=sm[:, 0:2], func=AF.Exp), reads=[ld], writes=[ld])
        P.op("dve", lambda e: e.tensor_tensor(out=sm[:, 2:3], in0=sm[:, 1:2], in1=sm[:, 0:1], op=ALU.subtract), reads=[ld], writes=[ld])
        P.op("dve", lambda e: e.tensor_scalar(out=sm[:, 2:3], in0=sm[:, 2:3], scalar1=lc[:, 0:1], scalar2=None, op0=ALU.add), reads=[ld], writes=[ld])
        P.op("dve", lambda e: e.tensor_scalar(out=gs, in0=gs, scalar1=lc[:, 1:2], scalar2=None, op0=ALU.mult), reads=[ld], writes=[ld])
        neglam = sm[:, 2:3]
        QTs = [(P.sb([128, 4, 256], BF16), Dep()) for _ in range(2)]
        PTs = [(P.sb([128, 256], BF16), Dep()) for _ in range(4)]
        OTs = [(P.sb([128, 4, 256], BF16), Dep()) for _ in range(2)]
        o0 = P.sb([128, 2, 128], F32)
        o1 = P.sb([128, 2, 128], F32)
        onb = P.sb([128, 2, 128], BF16)
        junk = P.sb([128, 128], F32)
        rr = P.sb([128, 8], F32)
        ed = Dep()
        P.set_slots(4, 8)
        qv = s["qA"].rearrange("(c p) t -> p c t", p=128)
        k = 0
        for tb in range(NB):
            QT, qd = QTs[tb % 2]
            OT, otd = OTs[tb % 2]
            P.dma(QT, qv[:, :, tb * TB:(tb + 1) * TB], reads=[self.dep("qA", tb)], writes=[qd])
            keys = list(range(NKT)) if tb < NB - 1 else [NLT, NLT + 1]
            for h in range(4):
                for m in range(2):
                    bk, bds = P.bankn((2 * h + m) % 4)
                    bv = bk.rearrange("p (q c) -> p q c", q=2)
                    P.op("dve", lambda e, bv=bv: e.memset(bv[:, :, 0:130], 0.0), writes=bds)
                    for ki, kt in enumerate(keys):
                        ps, psd = P.slot()
                        P.op("pe", lambda e, ps=ps, kt=kt, h=h, m=m, QT=QT: e.matmul(ps, lhsT=KT[m * 64:(m + 1) * 64, h, kt * 128:(kt + 1) * 128],
                                                                                     rhs=QT[m * 64:(m + 1) * 64, h, :], start=True, stop=True),
                             reads=[kd, qd], writes=[psd])
                        PT, ptd = PTs[k % 4]
                        k += 1
                        P.op("act", lambda e, PT=PT, ps=ps: e.activation(out=PT, in_=ps, func=AF.Exp, scale=0.125), reads=[psd], writes=[ptd])
                        for qs in range(2):
                            P.op("pe", lambda e, bv=bv, qs=qs, PT=PT, kt=kt, h=h, ki=ki, nk=len(keys): e.matmul(
                                bv[:, qs, 0:129], lhsT=PT[:, qs * 128:(qs + 1) * 128], rhs=Vaug[:, kt, h, 0:129],
                                start=False, stop=(ki == nk - 1), skip_group_check=True), reads=[ptd, vd], writes=bds)
                    if m == 0:
                        P.op("dve", lambda e, bv=bv: e.reciprocal(out=rr[:, 0:2], in_=bv[:, :, 128]), reads=bds, writes=[ed])
                        P.op("dve", lambda e, bv=bv: e.tensor_tensor(out=o0, in0=bv[:, :, 0:128], in1=rr[:, 0:2, None].broadcast_to([128, 2, 128]),
                                                                     op=ALU.mult), reads=bds + [ed], writes=[ed])
                    else:
                        P.op("dve", lambda e, bv=bv: e.reciprocal(out=rr[:, 2:4], in_=bv[:, :, 128]), reads=bds, writes=[ed])
                        P.op("dve", lambda e: e.tensor_scalar(out=rr[:, 2:4], in0=rr[:, 2:4], scalar1=neglam, scalar2=None, op0=ALU.mult),
                             reads=[ed, ld], writes=[ed])
                        P.op("dve", lambda e, bv=bv: e.tensor_tensor(out=o1, in0=bv[:, :, 0:128], in1=rr[:, 2:4, None].broadcast_to([128, 2, 128]),
                                                                     op=ALU.mult), reads=bds + [ed], writes=[ed])
                        P.op("pool", lambda e: e.tensor_tensor(out=o0, in0=o0, in1=o1, op=ALU.add), reads=[ed], writes=[ed])
                        P.op("pool", lambda e: e.memset(rr[:, 4:6], 0.0), reads=[ed], writes=[ed])
                        for qs in range(2):
                            P.op("act", lambda e, qs=qs: e.activation(out=junk, in_=o0[:, qs, :], func=AF.Square, accum_out=rr[:, 4 + qs:5 + qs]),
                                 reads=[ed], writes=[ed])
                        P.op("act", lambda e: e.activation(out=rr[:, 4:6], in_=rr[:, 4:6], func=AF.Sqrt, bias=EPS, scale=1.0 / 128), reads=[ed], writes=[ed])
                        P.op("dve", lambda e: e.reciprocal(out=rr[:, 4:6], in_=rr[:, 4:6]), reads=[ed], writes=[ed])
                        for qs in range(2):
                            P.op("dve", lambda e, qs=qs: e.scalar_tensor_tensor(out=onb[:, qs, :], in0=o0[:, qs, :], scalar=rr[:, 4 + qs:5 + qs], in1=gs,
                                                                                op0=ALU.mult, op1=ALU.mult), reads=[ed, ld], writes=[ed])
                        ps, psd = P.slot()
                        psb = ps.bitcast(BF16)
                        for qs in range(2):
                            P.op("pe", lambda e, psb=psb, qs=qs: e.transpose(psb[:, qs * 128:(qs + 1) * 128], onb[:, qs, :], self.ident),
                                 reads=[ed, pd], writes=[psd])
                        P.op("act", lambda e, OT=OT, h=h, psb=psb: e.copy(out=OT[:, h, :], in_=psb[:, 0:256]), reads=[psd], writes=[otd])
            P.dma(s["oT"][0:512, tb * TB:(tb + 1) * TB].rearrange("(c p) t -> p c t", p=128), OT, reads=[otd], writes=[self.dep("oT", tb)])

    def attnB_phase(self, P, l):
        P.phase()
        s, i, pd = self.s, self.i, self.pd
        T, NKT, NLT, NB = self.T, self.NKT, self.NLT, self.NB
        scale = 192.0 ** -0.5
        KT = P.sb([128, 4, T], BF16)
        KP = P.sb([128, T], BF16)
        kd = Dep()
        Vaug = P.sb([128, NKT, 4, 130], BF16)
        vd = Dep()
        P.dma(KT, s["kBn"].rearrange("(c p) t -> p c t", p=128), reads=self.bdeps("kBn"), writes=[kd])
        P.dma(KP, s["kpe2"], reads=self.bdeps("kpe2"), writes=[kd])
        self.load_vaug(P, Vaug, vd, "vB", 4, 128)
        QTs = [(P.sb([128, 6, 256], BF16), Dep()) for _ in range(2)]
        PTs = [(P.sb([128, 256], BF16), Dep()) for _ in range(4)]
        OTs = [(P.sb([128, 4, 256], BF16), Dep()) for _ in range(2)]
        onb = P.sb([128, 2, 128], BF16)
        rr = P.sb([128, 8], F32)
        ed = Dep()
        P.set_slots(4, 8)
        qv = s["qBn"].rearrange("(c p) t -> p c t", p=128)
        qpv = s["qBpe"].rearrange("(c p) t -> p c t", p=128)
        k = 0
        for tb in range(NB):
            QT, qd = QTs[tb % 2]
            OT, otd = OTs[tb % 2]
            P.dma(QT[:, 0:4, :], qv[:, :, tb * TB:(tb + 1) * TB], reads=[self.dep("qBn", tb)], writes=[qd])
            P.dma(QT[:, 4:6, :], qpv[:, :, tb * TB:(tb + 1) * TB], reads=[self.dep("qBpe", tb)], writes=[qd])
            keys = list(range(NKT)) if tb < NB - 1 else [NLT, NLT + 1]
            for h in range(4):
                bk, bds = P.bankn(h % 4)
                bv = bk.rearrange("p (q c) -> p q c", q=2)
                P.op("dve", lambda e, bv=bv: e.memset(bv[:, :, 0:130], 0.0), writes=bds)
                pb = (h % 2) * 64
                for ki, kt in enumerate(keys):
                    ps, psd = P.slot()
                    P.op("pe", lambda e, ps=ps, kt=kt, h=h, QT=QT: e.matmul(ps, lhsT=KT[:, h, kt * 128:(kt + 1) * 128], rhs=QT[:, h, :],
                                                                            start=True, stop=False), reads=[kd, qd], writes=[psd])
                    P.op("pe", lambda e, ps=ps, kt=kt, h=h, QT=QT, pb=pb: e.matmul(ps, lhsT=KP[pb:pb + 64, kt * 128:(kt + 1) * 128],
                                                                                   rhs=QT[pb:pb + 64, 4 + h // 2, :], start=False, stop=True),
                         reads=[kd, qd], writes=[psd])
                    PT, ptd = PTs[k % 4]
                    k += 1
                    P.op("act", lambda e, PT=PT, ps=ps: e.activation(out=PT, in_=ps, func=AF.Exp, scale=scale), reads=[psd], writes=[ptd])
                    for qs in range(2):
                        P.op("pe", lambda e, bv=bv, qs=qs, PT=PT, kt=kt, h=h, ki=ki, nk=len(keys): e.matmul(
                            bv[:, qs, 0:129], lhsT=PT[:, qs * 128:(qs + 1) * 128], rhs=Vaug[:, kt, h, 0:129],
                            start=False, stop=(ki == nk - 1), skip_group_check=True), reads=[ptd, vd], writes=bds)
                P.op("dve", lambda e, bv=bv: e.reciprocal(out=rr[:, 0:2], in_=bv[:, :, 128]), reads=bds, writes=[ed])
                P.op("dve", lambda e, bv=bv: e.tensor_tensor(out=onb, in0=bv[:, :, 0:128], in1=rr[:, 0:2, None].broadcast_to([128, 2, 128]),
                                                             op=ALU.mult), reads=bds + [ed], writes=[ed])
                ps, psd = P.slot()
                psb = ps.bitcast(BF16)
                for qs in range(2):
                    P.op("pe", lambda e, psb=psb, qs=qs: e.transpose(psb[:, qs * 128:(qs + 1) * 128], onb[:, qs, :], self.ident),
                         reads=[ed, pd], writes=[psd])
                P.op("act", lambda e, OT=OT, h=h, psb=psb: e.copy(out=OT[:, h, :], in_=psb[:, 0:256]), reads=[psd], writes=[otd])
            P.dma(s["oT"][512:1024, tb * TB:(tb + 1) * TB].rearrange("(c p) t -> p c t", p=128), OT, reads=[otd], writes=[self.dep("oT", tb)])

    def attnC_phase(self, P, l):
        P.phase()
        s, i, pd = self.s, self.i, self.pd
        T, NKT, NLT, NB = self.T, self.NKT, self.NLT, self.NB
        KT = P.sb([128, 2, T], BF16)
        kd = Dep()
        Vaug = P.sb([128, NKT, 2, 66], BF16)
        vd = Dep()
        P.dma(KT, s["kC2"].rearrange("(c p) t -> p c t", p=128), reads=self.bdeps("kC2"), writes=[kd])
        self.load_vaug(P, Vaug, vd, "vC", 2, 64)
        cm = P.sb([128, 256], F32)
        cmb = P.sb([128, 256], BF16)
        sk = P.sb([128, 8], F32)
        ld = Dep()
        P.dma(cm, i["cmask"][:, :], writes=[ld])
        P.op("dve", lambda e: e.tensor_copy(out=cmb, in_=cm), reads=[ld], writes=[ld])
        P.dma(sk, i["sink"][l][0:1, :].broadcast_to([128, 8]), writes=[ld])
        P.op("act", lambda e: e.activation(out=sk, in_=sk, func=AF.Exp), reads=[ld], writes=[ld])
        QTs = [(P.sb([128, 4, 256], BF16), Dep()) for _ in range(2)]
        PTs = [(P.sb([128, 128], BF16), Dep()) for _ in range(4)]
        OTs = [(P.sb([128, 4, 256], BF16), Dep()) for _ in range(2)]
        onbs = [(P.sb([128, 512], BF16), Dep()) for _ in range(2)]
        rr = P.sb([128, 8], F32)
        ed = Dep()
        qv = s["qC"].rearrange("(c p) t -> p c t", p=128)
        P.set_slots(4, 8)
        k = 0
        for tb in range(NB):
            QT, qd = QTs[tb % 2]
            OT, otd = OTs[tb % 2]
            P.dma(QT, qv[:, :, tb * TB:(tb + 1) * TB], reads=[self.dep("qC", tb)], writes=[qd])
            for qs in range(2):
                qt = tb * 2 + qs
                if tb < NB - 1:
                    keys = []
                    if qt - 1 >= 0:
                        keys.append((qt - 1, 0))
                    keys.append((qt, None))
                    if qt + 1 < NLT:
                        keys.append((qt + 1, 1))
                    keys += [(NLT, None), (NLT + 1, None)]
                else:
                    keys = [(NLT, None), (NLT + 1, None)]
                onb, ond = onbs[qt % 2]
                for hq in range(8):
                    kh = hq // 4
                    pb = (hq % 2) * 64
                    acc, accd = P.slotn(2 * (hq % 4))
                    P.op("dve", lambda e, acc=acc: e.memset(acc[:, 0:66], 0.0), writes=[accd])
                    for ki, (kt, msk) in enumerate(keys):
                        ps, psd = P.slot()
                        P.op("pe", lambda e, ps=ps, kt=kt, kh=kh, pb=pb, hq=hq, QT=QT, qs=qs: e.matmul(
                            ps[:, 0:128], lhsT=KT[pb:pb + 64, kh, kt * 128:(kt + 1) * 128], rhs=QT[pb:pb + 64, hq // 2, qs * 128:(qs + 1) * 128],
                            start=True, stop=True), reads=[kd, qd], writes=[psd])
                        PT, ptd = PTs[k % 4]
                        k += 1
                        P.op("act", lambda e, PT=PT, ps=ps: e.activation(out=PT, in_=ps[:, 0:128], func=AF.Exp, scale=0.125), reads=[psd], writes=[ptd])
                        if msk is not None:
                            P.op("pool", lambda e, PT=PT, msk=msk: e.tensor_tensor(out=PT, in0=PT, in1=cmb[:, msk * 128:(msk + 1) * 128], op=ALU.mult),
                                 reads=[ptd, ld], writes=[ptd])
                        P.op("pe", lambda e, acc=acc, PT=PT, kt=kt, kh=kh, ki=ki, nk=len(keys): e.matmul(
                            acc[:, 0:65], lhsT=PT, rhs=Vaug[:, kt, kh, 0:65], start=False, stop=(ki == nk - 1), skip_group_check=True), reads=[ptd, vd], writes=[accd])
                    P.op("dve", lambda e, acc=acc, hq=hq: e.tensor_tensor(out=rr[:, 0:1], in0=acc[:, 64:65], in1=sk[:, hq:hq + 1], op=ALU.add),
                         reads=[accd, ld], writes=[ed])
                    P.op("dve", lambda e: e.reciprocal(out=rr[:, 0:1], in_=rr[:, 0:1]), reads=[ed], writes=[ed])
                    P.op("dve", lambda e, acc=acc, hq=hq, onb=onb: e.tensor_scalar(out=onb[:, hq * 64:(hq + 1) * 64], in0=acc[:, 0:64], scalar1=rr[:, 0:1],
                                                                                   scalar2=None, op0=ALU.mult), reads=[accd, ed], writes=[ond])
                self.transpose_out(P, onb, ond, 4, OT, otd, qs)
            P.dma(s["oT"][1024:1536, tb * TB:(tb + 1) * TB].rearrange("(c p) t -> p c t", p=128), OT, reads=[otd], writes=[self.dep("oT", tb)])

    def attnD_phase(self, P, l):
        P.phase()
        s, i, pd = self.s, self.i, self.pd
        T, NKT, NLT, NB = self.T, self.NKT, self.NLT, self.NB
        R = self.SEQ // GRID_W
        scale = 128.0 ** -0.5
        KT = P.sb([128, 4, T], BF16)
        kd = Dep()
        Vaug = P.sb([128, NKT, 4, 130], BF16)
        vd = Dep()
        P.dma(KT, s["kD"].rearrange("(c p) t -> p c t", p=128), reads=self.bdeps("kD"), writes=[kd])
        self.load_vaug(P, Vaug, vd, "vD", 4, 128)
        ep = P.sb([60, 160], F32)
        TZ = P.sb([128, 60, 64], F32)
        TZb = P.sb([128, 60, 64], BF16)
        colm = P.sb([128, 64], F32)
        td = Dep()
        epd = self.dep("EP")
        P.op("pool", lambda e: e.memset(ep, 0.0), writes=[td])
        P.dma(ep[:, 64:95], i["rpbr"][l], reads=[td], writes=[td])
        P.op("act", lambda e: e.activation(out=ep[:, 64:95], in_=ep[:, 64:95], func=AF.Exp), reads=[td], writes=[td])
        P.dma(s["EP"], ep, reads=[td], writes=[epd])
        P.dma(colm, i["colm"][:, :], writes=[td])
        for kc in range(64):
            for half in range(2):
                p = half * 64 + kc
                P.dma(TZ[p:p + 1, :, :], s["EP"][None, :, 79 - kc:79 - kc + 64], reads=[epd], writes=[td])
        P.op("dve", lambda e: e.tensor_tensor(out=TZb, in0=TZ, in1=colm[:, None, :].broadcast_to([128, 60, 64]), op=ALU.mult), reads=[td], writes=[td])
        QTs = [(P.sb([128, 4, 256], BF16), Dep()) for _ in range(2)]
        PTs = [(P.sb([128, 128], BF16), Dep()) for _ in range(4)]
        OTs = [(P.sb([128, 4, 256], BF16), Dep()) for _ in range(2)]
        onbs = [(P.sb([128, 512], BF16), Dep()) for _ in range(2)]
        rr = P.sb([128, 8], F32)
        ed = Dep()
        qv = s["qD"].rearrange("(c p) t -> p c t", p=128)

        def rs_(r):
            return min(max(r - 4, 0), R - 8)
        P.set_slots(4, 8)
        k = 0
        for tb in range(NB):
            QT, qd = QTs[tb % 2]
            OT, otd = OTs[tb % 2]
            P.dma(QT, qv[:, :, tb * TB:(tb + 1) * TB], reads=[self.dep("qD", tb)], writes=[qd])
            for qs in range(2):
                qt = tb * 2 + qs
                if tb < NB - 1:
                    lo = rs_(2 * qt) // 2
                    hi = (rs_(2 * qt + 1) + 7) // 2
                    keys = [(kt, True) for kt in range(lo, hi + 1)] + [(NLT, False), (NLT + 1, False)]
                else:
                    keys = [(NLT, False), (NLT + 1, False)]
                onb, ond = onbs[qt % 2]
                for h in range(4):
                    acc, accd = P.slotn(2 * h)
                    P.op("dve", lambda e, acc=acc: e.memset(acc[:, 0:130], 0.0), writes=[accd])
                    for ki, (kt, nb) in enumerate(keys):
                        ps, psd = P.slot()
                        P.op("pe", lambda e, ps=ps, kt=kt, h=h, QT=QT, qs=qs: e.matmul(
                            ps[:, 0:128], lhsT=KT[:, h, kt * 128:(kt + 1) * 128], rhs=QT[:, h, qs * 128:(qs + 1) * 128], start=True, stop=True),
                            reads=[kd, qd], writes=[psd])
                        PT, ptd = PTs[k % 4]
                        k += 1
                        P.op("act", lambda e, PT=PT, ps=ps: e.activation(out=PT, in_=ps[:, 0:128], func=AF.Exp, scale=scale), reads=[psd], writes=[ptd])
                        if nb:
                            for a in range(2):
                                for b in range(2):
                                    kr, r = 2 * kt + a, 2 * qt + b
                                    quad = PT[a * 64:(a + 1) * 64, b * 64:(b + 1) * 64]
                                    eng = "dve" if (a + b) % 2 == 0 else "pool"
                                    if rs_(r) <= kr < rs_(r) + 8:
                                        idx = h * 15 + (kr - r + 7)
                                        P.op(eng, lambda e, quad=quad, a=a, idx=idx: e.tensor_tensor(out=quad, in0=quad, in1=TZb[a * 64:(a + 1) * 64, idx, :],
                                                                                                     op=ALU.mult), reads=[ptd, td], writes=[ptd])
                                    else:
                                        P.op(eng, lambda e, quad=quad: e.memset(quad, 0.0), reads=[ptd], writes=[ptd])
                        P.op("pe", lambda e, acc=acc, PT=PT, kt=kt, h=h, ki=ki, nk=len(keys): e.matmul(
                            acc[:, 0:129], lhsT=PT, rhs=Vaug[:, kt, h, 0:129], start=False, stop=(ki == nk - 1), skip_group_check=True), reads=[ptd, vd], writes=[accd])
                    P.op("dve", lambda e, acc=acc: e.reciprocal(out=rr[:, 0:1], in_=acc[:, 128:129]), reads=[accd], writes=[ed])
                    P.op("dve", lambda e, acc=acc, h=h, onb=onb: e.tensor_scalar(out=onb[:, h * 128:(h + 1) * 128], in0=acc[:, 0:128], scalar1=rr[:, 0:1],
                                                                                 scalar2=None, op0=ALU.mult), reads=[accd, ed], writes=[ond])
                self.transpose_out(P, onb, ond, 4, OT, otd, qs)
            P.dma(s["oT"][1536:2048, tb * TB:(tb + 1) * TB].rearrange("(c p) t -> p c t", p=128), OT, reads=[otd], writes=[self.dep("oT", tb)])

    def merge_phase(self, P, l):
        P.phase()
        s, i, pd = self.s, self.i, self.pd
        NB = self.NB
        hbs = [(P.sb([128, 16, 256], BF16), Dep()) for _ in range(2)]
        obs = [(P.sb([128, 16, 256], BF16), Dep()) for _ in range(2)]
        xts = [(P.sb([128, 16, 256], F32), Dep()) for _ in range(2)]
        wgs = [(P.sb([128, 4, 16, 128], BF16), Dep()) for _ in range(2)]
        wbs = [(P.sb([128, 4, 4, 128], BF16), Dep()) for _ in range(2)]
        wos = [(P.sb([128, 4, 16, 128], BF16), Dep()) for _ in range(2)]
        mg = P.sb([128, 16, 256], BF16)
        mgd = Dep()
        accs = [(P.sb([128, 256], F32), Dep()) for _ in range(4)]
        gts = [(P.sb([128, 256], F32), Dep()) for _ in range(4)]
        tms = [(P.sb([128, 256], F32), Dep()) for _ in range(2)]
        bgt = P.sb([128, 64], F32)
        bd = Dep()
        P.dma(bgt, i["bg"][l], writes=[bd])
        hTv = s["hT"].rearrange("(c p) t -> p c t", p=128)
        oTv = s["oT"].rearrange("(c p) t -> p c t", p=128)
        xsrc = (self.i["xT0"] if l == 0 else s["xT"]).rearrange("(c p) t -> p c t", p=128)
        xsn = "xT0" if l == 0 else "xT"
        xdst = s["xT"].rearrange("(c p) t -> p c t", p=128)
        cnt = {"g": 0, "t": 0}
        for tb in range(NB):
            hb, hd = hbs[tb % 2]
            ob, od = obs[tb % 2]
            xt, xd = xts[tb % 2]
            j = 1 if tb == NB - 1 else 0
            sl = slice(tb * TB, (tb + 1) * TB)
            P.dma(hb, hTv[:, :, sl], reads=[self.dep("hT", tb)], writes=[hd])
            P.dma(ob, oTv[:, :, sl], reads=[self.dep("oT", tb)], writes=[od])
            P.dma(xt, xsrc[:, :, sl], reads=[self.dep(xsn, tb)], writes=[xd])
            for ng in range(4):
                for br in range(4):
                    wg, wgd = wgs[cnt["g"] % 2]
                    wb, wbd = wbs[cnt["g"] % 2]
                    cnt["g"] += 1
                    P.dma(wg, s["wgt"][:, br * 16 + ng * 4:br * 16 + ng * 4 + 4, :, :], reads=[self.dep("wgb")], writes=[wgd])
                    P.dma(wb, s["wbt"][:, br * 16 + ng * 4:br * 16 + ng * 4 + 4, :, :], reads=[self.dep("wbb")], writes=[wbd])
                    for jj in range(4):
                        n = ng * 4 + jj
                        psg, pgd = P.slot()
                        self.mm_chain(P, psg, [pgd], [(wg[:, jj, kc, :], hb[:, kc, :]) for kc in range(KC)], [wgd, hd])
                        gt, gd = gts[jj]
                        P.op("act", lambda e, gt=gt, psg=psg, br=br, n=n: e.activation(out=gt, in_=psg, func=AF.Sigmoid, bias=bgt[:, br * 16 + n:br * 16 + n + 1]),
                             reads=[pgd, bd], writes=[gd])
                        psb, pbd = P.slot()
                        self.mm_chain(P, psb, [pbd], [(wb[:, jj, kc, :], ob[:, br * 4 + kc, :]) for kc in range(4)], [wbd, od])
                        acc, ad = accs[jj]
                        if br == 0:
                            P.op("dve", lambda e, acc=acc, psb=psb, gt=gt: e.tensor_tensor(out=acc, in0=psb, in1=gt, op=ALU.mult), reads=[pbd, gd], writes=[ad])
                        else:
                            tm, tmd = tms[cnt["t"] % 2]
                            cnt["t"] += 1
                            P.op("dve", lambda e, tm=tm, psb=psb, gt=gt: e.tensor_tensor(out=tm, in0=psb, in1=gt, op=ALU.mult), reads=[pbd, gd], writes=[tmd])
                            if br < 3:
                                P.op("pool", lambda e, acc=acc, tm=tm: e.tensor_tensor(out=acc, in0=acc, in1=tm, op=ALU.add), reads=[tmd, ad], writes=[ad])
                            else:
                                P.op("pool", lambda e, acc=acc, tm=tm, n=n: e.tensor_tensor(out=mg[:, n, :], in0=acc, in1=tm, op=ALU.add),
                                     reads=[tmd, ad], writes=[mgd])
            for ng in range(4):
                wo, wod = wos[ng % 2]
                P.dma(wo, s["wot"][:, ng * 4:ng * 4 + 4, :, :], reads=[self.dep("wob")], writes=[wod])
                for jj in range(4):
                    n = ng * 4 + jj
                    ps, psd = P.slot()
                    self.mm_chain(P, ps, [psd], [(wo[:, jj, kc, :], mg[:, kc, :]) for kc in range(KC)], [wod, mgd])
                    P.op("dve", lambda e, xt=xt, ps=ps, n=n, j=j: e.scalar_tensor_tensor(out=xt[:, n, :], in0=ps, scalar=self.mod[:, 32 + n, j:j + 1], in1=xt[:, n, :],
                                                                                    op0=ALU.mult, op1=ALU.add), reads=[psd, pd, xd], writes=[xd])
            P.dma(xdst[:, :, sl], xt, reads=[xd], writes=[self.dep("xT", tb)])

    def peer_phase(self, P, l):
        P.phase()
        s, i, pd = self.s, self.i, self.pd
        NB = self.NB
        NE = 128
        CfT = P.sb([128, NE, 256], BF16)
        cfd = [Dep() for _ in range(NE)]
        hbs = [(P.sb([128, 16, 256], BF16), Dep()) for _ in range(1)]
        qT = P.sb([128, 16, 256], BF16)
        qdp = Dep()
        raws = [(P.sb([128, 8192], BF16), Dep()) for _ in range(2)]
        wqs = [(r.rearrange("p (c k n) -> p c k n", c=4, k=16), d_) for (r, d_) in raws]
        uts = wqs
        vts = [(r[:, 0:4096].rearrange("p (c n) -> p c n", c=4), d_) for (r, d_) in raws]
        ky = P.sb([128, 16, 128], BF16)
        kyd = Dep()
        ssb = P.sb([128, 16, 128], F32)
        s2 = P.sb([128, 256], F32)
        top = P.sb([128, 16, 16], F32)
        cand = P.sb([128, 8, 256], F32)
        kyf = cand.rearrange("p a b -> p (a b)")
        P.dma(kyf, i["keysT"][l], writes=[kyd])
        P.op("dve", lambda e: e.tensor_copy(out=ky, in_=kyf.rearrange("p (a b) -> p a b", a=16)), reads=[kyd], writes=[kyd])
        ctop = P.sb([128, 8, 16], F32)
        et = P.sb([128, 8, 16], F32)
        sm = P.sb([128, 32], F32)
        sd_ = Dep()
        Sp = [(P.sb([128, 16, 128], F32), Dep()) for _ in range(2)]
        Eb = [(P.sb([128, 16, 128], BF16), Dep()) for _ in range(2)]
        Wh = [(P.sb([128, 16, 128], BF16), Dep()) for _ in range(2)]
        Wacc = P.sb([128, 16, 128], F32)
        Wb = P.sb([128, 16, 128], BF16)
        wad = Dep()
        gl = [(P.sb([128, 256], BF16), Dep()) for _ in range(2)]
        xts = [(P.sb([128, 8, 256], F32), Dep()) for _ in range(1)]
        wbd_ = Dep()
        hTv = s["hT"].rearrange("(c p) t -> p c t", p=128)
        xv = s["xT"].rearrange("(c p) t -> p c t", p=128)
        thr, mx, bias_, lz = sm[:, 0:8], sm[:, 8:16], sm[:, 16:24], sm[:, 24:32]
        cnt = {"s": 0, "x": 0, "w": 0}
        for tb in range(NB):
            hb, hd = hbs[0]
            j = 1 if tb == NB - 1 else 0
            sl = slice(tb * TB, (tb + 1) * TB)
            P.dma(hb, hTv[:, :, sl], reads=[self.dep("hT", tb)], writes=[hd])
            P.set_slots(0, 8)
            for ng in range(4):
                wq, wqd = wqs[cnt["w"] % 2]
                cnt["w"] += 1
                P.dma(wq, s["wqt"][:, ng * 4:ng * 4 + 4, :, :], reads=[self.dep("wqb")], writes=[wqd])
                for jj in range(4):
                    ps, psd = P.slot()
                    self.mm_chain(P, ps, [psd], [(wq[:, jj, kc, :], hb[:, kc, :]) for kc in range(KC)], [wqd, hd])
                    P.op("act", lambda e, ps=ps, n=ng * 4 + jj: e.copy(out=qT[:, n, :], in_=ps), reads=[psd], writes=[qdp])
            for tt in range(2):
                for g4 in range(4):
                    bk, bds = P.bank()
                    for q4 in range(4):
                        hp = g4 * 4 + q4
                        P.op("pe", lambda e, bk=bk, q4=q4, hp=hp, tt=tt: e.matmul(bk[:, q4 * 128:(q4 + 1) * 128], lhsT=qT[:, hp, tt * 128:(tt + 1) * 128],
                                                                                   rhs=ky[:, hp, :], start=True, stop=True), reads=[qdp, kyd], writes=bds)
                    P.op("act", lambda e, bk=bk, g4=g4: e.copy(out=ssb[:, g4 * 4:(g4 + 1) * 4, :], in_=bk.rearrange("p (a b) -> p a b", a=4)),
                         reads=bds, writes=[sd_])
                for hp in range(16):
                    P.op("dve", lambda e, hp=hp: e.max(out=top[:, hp, 0:8], in_=ssb[:, hp, :]), reads=[sd_], writes=[sd_])
                    P.op("dve", lambda e, hp=hp: e.match_replace(out=s2[:, 0:128], in_to_replace=top[:, hp, 0:8], in_values=ssb[:, hp, :], imm_value=-1e30),
                         reads=[sd_], writes=[sd_])
                    P.op("dve", lambda e, hp=hp: e.max(out=top[:, hp, 8:16], in_=s2[:, 0:128]), reads=[sd_], writes=[sd_])
                topv = top.rearrange("p (h two) k -> p h two k", two=2)
                for h in range(8):
                    P.op("dve", lambda e, h=h: e.tensor_tensor(out=cand[:, h, :].rearrange("p (a b) -> p a b", a=16),
                                                               in0=topv[:, h, 0, :, None].broadcast_to([128, 16, 16]),
                                                               in1=topv[:, h, 1, None, :].broadcast_to([128, 16, 16]), op=ALU.add), reads=[sd_], writes=[sd_])
                    P.op("dve", lambda e, h=h: e.max(out=ctop[:, h, 0:8], in_=cand[:, h, :]), reads=[sd_], writes=[sd_])
                    P.op("dve", lambda e, h=h: e.match_replace(out=s2, in_to_replace=ctop[:, h, 0:8], in_values=cand[:, h, :], imm_value=-1e30),
                         reads=[sd_], writes=[sd_])
                    P.op("dve", lambda e, h=h: e.max(out=ctop[:, h, 8:16], in_=s2), reads=[sd_], writes=[sd_])
                P.op("dve", lambda e: e.tensor_copy(out=mx, in_=ctop[:, :, 0]), reads=[sd_], writes=[sd_])
                P.op("dve", lambda e: e.tensor_copy(out=thr, in_=ctop[:, :, 15]), reads=[sd_], writes=[sd_])
                P.op("dve", lambda e: e.tensor_tensor(out=et, in0=ctop, in1=mx[:, :, None].broadcast_to([128, 8, 16]), op=ALU.subtract), reads=[sd_], writes=[sd_])
                P.op("act", lambda e: e.activation(out=et, in_=et, func=AF.Exp), reads=[sd_], writes=[sd_])
                P.op("dve", lambda e: e.tensor_reduce(out=lz, in_=et, axis=AX.X, op=ALU.add), reads=[sd_], writes=[sd_])
                P.op("act", lambda e: e.activation(out=lz, in_=lz, func=AF.Ln), reads=[sd_], writes=[sd_])
                P.op("dve", lambda e: e.tensor_tensor(out=bias_, in0=thr, in1=mx, op=ALU.subtract), reads=[sd_], writes=[sd_])
                P.op("dve", lambda e: e.tensor_tensor(out=bias_, in0=bias_, in1=lz, op=ALU.subtract), reads=[sd_], writes=[sd_])
                for ig in range(8):
                    i0 = ig * 16
                    for h in range(8):
                        sp, spd = Sp[cnt["s"] % 2]
                        eb, ebd = Eb[cnt["s"] % 2]
                        wh, whd = Wh[cnt["s"] % 2]
                        cnt["s"] += 1
                        P.op("dve", lambda e, sp=sp, h=h, i0=i0: e.scalar_tensor_tensor(
                            out=sp, in0=ssb[:, 2 * h, i0:i0 + 16, None].broadcast_to([128, 16, 128]), scalar=thr[:, h:h + 1],
                            in1=ssb[:, 2 * h + 1, None, :].broadcast_to([128, 16, 128]), op0=ALU.subtract, op1=ALU.add), reads=[sd_], writes=[spd])
                        P.op("act", lambda e, sp=sp, eb=eb, h=h: e.activation(out=eb, in_=sp, func=AF.Exp, bias=bias_[:, h:h + 1]), reads=[spd, sd_], writes=[ebd])
                        if h == 0:
                            P.op("dve", lambda e, sp=sp, eb=eb: e.scalar_tensor_tensor(out=Wacc, in0=sp, scalar=0.0, in1=eb, op0=ALU.is_ge, op1=ALU.mult),
                                 reads=[spd, ebd], writes=[wad])
                        else:
                            P.op("dve", lambda e, sp=sp, eb=eb, wh=wh: e.scalar_tensor_tensor(out=wh, in0=sp, scalar=0.0, in1=eb, op0=ALU.is_ge, op1=ALU.mult),
                                 reads=[spd, ebd], writes=[whd])
                            P.op("pool", lambda e, wh=wh: e.tensor_tensor(out=Wacc, in0=Wacc, in1=wh, op=ALU.add), reads=[whd, wad], writes=[wad])
                    P.op("act", lambda e: e.copy(out=Wb, in_=Wacc), reads=[wad], writes=[wbd_])
                    for c0 in range(0, 16, 4):
                        ps, psd = P.slot()
                        psb = ps.bitcast(BF16)
                        for c in range(4):
                            P.op("pe", lambda e, psb=psb, c=c, c0=c0: e.transpose(psb[:, c * 128:(c + 1) * 128], Wb[:, c0 + c, :], self.ident),
                                 reads=[wbd_, pd], writes=[psd])
                        ec = i0 + c0
                        P.op("act", lambda e, psb=psb, ec=ec, tt=tt: e.copy(out=CfT[:, ec:ec + 4, tt * 128:(tt + 1) * 128],
                                                                             in_=psb[:, 0:512].rearrange("p (c q) -> p c q", c=4)),
                             reads=[psd], writes=cfd[ec:ec + 4])
            for eg in range(NE // 4):
                ut, utd = uts[cnt["w"] % 2]
                cnt["w"] += 1
                P.dma(ut, s["uTt"][:, eg * 4:eg * 4 + 4, :, :], reads=[self.dep("uTb")], writes=[utd])
                for jj in range(4):
                    ec = eg * 4 + jj
                    ps, psd = P.slot()
                    self.mm_chain(P, ps, [psd], [(ut[:, jj, kc, :], hb[:, kc, :]) for kc in range(KC)], [utd, hd])
                    g, gd = gl[ec % 2]
                    P.op("act", lambda e, g=g, ps=ps: e.activation(out=g, in_=ps, func=AF.Gelu), reads=[psd], writes=[gd])
                    eng = "dve" if ec % 2 == 0 else "pool"
                    P.op(eng, lambda e, g=g, ec=ec: e.tensor_tensor(out=CfT[:, ec, :], in0=CfT[:, ec, :], in1=g, op=ALU.mult), reads=[gd, cfd[ec]], writes=[cfd[ec]])
            for dh in range(2):
                base = 0 if dh == 0 else 8
                accs = [P.slotn(base + q) for q in range(8)]
                for q in range(0, 8, 2):
                    bk_, bd_ = P.bankn((base + q) // 2)
                    P.op("dve", lambda e, bk_=bk_: e.memset(bk_, 0.0), writes=bd_)
                xt, xd = xts[0]
                cnt["x"] += 1
                P.dma(xt, xv[:, dh * 8:(dh + 1) * 8, sl], reads=[self.dep("xT", tb)], writes=[xd])
                for eg in range(NE // 4):
                    vt, vtd = vts[cnt["w"] % 2]
                    cnt["w"] += 1
                    P.dma(vt, s["vt"][:, eg, dh, :, :], reads=[self.dep("vb")], writes=[vtd])
                    for jj in range(4):
                        ec = eg * 4 + jj
                        for dc in range(8):
                            ps, psd = accs[dc]
                            P.op("pe", lambda e, ps=ps, vt=vt, jj=jj, dc=dc, ec=ec: e.matmul(ps, lhsT=vt[:, jj, dc * 128:(dc + 1) * 128], rhs=CfT[:, ec, :],
                                                                                          start=False, stop=(ec == NE - 1), skip_group_check=True), reads=[vtd, cfd[ec]], writes=[psd])
                for dc in range(8):
                    ps, psd = accs[dc]
                    n = dh * 8 + dc
                    P.op("dve", lambda e, xt=xt, ps=ps, dc=dc, n=n, j=j: e.scalar_tensor_tensor(out=xt[:, dc, :], in0=ps, scalar=self.mod[:, 80 + n, j:j + 1], in1=xt[:, dc, :],
                                                                                           op0=ALU.mult, op1=ALU.add), reads=[psd, pd, xd], writes=[xd])
                P.dma(xv[:, dh * 8:(dh + 1) * 8, sl], xt, reads=[xd], writes=[self.dep("xT", tb)])

    def build(self, stop_after=None):
        nc = self.nc
        self.declare()
        with ExitStack() as es:
            P = Prog(nc, es)
            self.persist(P)
            fn = P.sb([128, 16, 1], F32)
            P.dma(fn, self.i["fnorm"].rearrange("p (c o) -> p c o", o=1), writes=[self.pd])
            P.sb_base = P.sb_off
            done = False
            for l in range(self.L):
                steps = [("cast", lambda l=l: self.cast_phase(P, l)), ("adaln", lambda l=l: self.adaln_phase(P, l)),
                         ("norm1", lambda l=l: self.norm_phase(P, self.i["xT0"] if l == 0 else self.s["xT"], "xT0" if l == 0 else "xT",
                                                               self.a1, self.mod[:, 0:16, :], self.s["hT"], "hT")),
                         ("proj", lambda l=l: self.proj_phase(P, l)), ("A", lambda l=l: self.attnA_phase(P, l)),
                         ("B", lambda l=l: self.attnB_phase(P, l)), ("C", lambda l=l: self.attnC_phase(P, l)),
                         ("D", lambda l=l: self.attnD_phase(P, l)), ("merge", lambda l=l: self.merge_phase(P, l)),
                         ("norm2", lambda l=l: self.norm_phase(P, self.s["xT"], "xT", self.a2, self.mod[:, 48:64, :], self.s["hT"], "hT")),
                         ("peer", lambda l=l: self.peer_phase(P, l))]
                for (nm, f) in steps:
                    f()
                    if stop_after == (l, nm):
                        done = True
                        break
                if done:
                    break
            if not done:
                self.norm_phase(P, self.s["xT"], "xT", fn, None, self.yT, "yT", out_f32=True, nblocks=self.NB - 1)
            P.barrier()
            self.n_instr = sum(len(v) for v in P.ops.values())
            P.emit()
        return nc


def host_inputs(SEQ, DEPTH, l0, xT0, x, c, ctx, c_ctx, ada_w, ada_b, norm_mix, norm_ffn, w_in, a_lam_q1, a_lam_k1, a_lam_q2, a_lam_k2,
                a_subln, b_qa_norm, b_w_uq, b_kva_norm, b_w_ukv, c_sink, d_rpb, w_branch, w_gate, b_gate, w_out,
                peer_wq, peer_keys, peer_u, peer_v, final_norm):
    L = DEPTH
    f = lambda a: np.ascontiguousarray(np.asarray(a, dtype=np.float32))
    m = {}
    m["xT0"] = f(np.concatenate([np.asarray(x)[0], np.asarray(ctx)[0]], axis=0).T) if xT0 is None else f(xT0)
    lam_init = 0.8 - 0.6 * math.exp(-0.3 * l0)
    m["lamc"] = np.array([[-lam_init, 1.0 - lam_init]], np.float32)
    cc = np.stack([np.asarray(c)[0], np.asarray(c_ctx)], axis=-1)
    m["cc"] = f(cc.reshape(16, 128, 2).transpose(1, 0, 2).reshape(128, 32))
    m["ada_w"] = f(ada_w[l0:l0 + L])
    m["adab"] = f(np.stack([pcl(np.asarray(ada_b[l]), 96) for l in range(l0, l0 + L)]))
    m["nmix"] = f(np.stack([pcl(np.asarray(norm_mix[l]), 16) for l in range(l0, l0 + L)]))
    m["nffn"] = f(np.stack([pcl(np.asarray(norm_ffn[l]), 16) for l in range(l0, l0 + L)]))
    m["wproj"] = f(np.stack([build_wproj(np.asarray(w_in[l])) for l in range(l0, l0 + L)]))
    m["lamp"] = f(np.stack([np.concatenate([a_lam_q1[l], a_lam_q2[l], a_lam_k1[l], a_lam_k2[l]])[None, :] for l in range(l0, l0 + L)]))
    m["subln"] = f(np.asarray(a_subln)[:L, None, :])
    m["qan"] = f(np.stack([pcl(np.asarray(b_qa_norm[l]), 4) for l in range(l0, l0 + L)]))
    wuq = np.asarray(b_w_uq)[l0:l0 + L].reshape(L, 512, 4, 192)
    nope = wuq[:, :, :, 0:128].reshape(L, 512, 512)
    pe = wuq[:, :, :, 128:192].reshape(L, 512, 256)
    pes = np.stack([_swap64(pe[l]) for l in range(L)])
    m["wuq2"] = f(np.concatenate([nope, pe, pes], axis=2))
    m["kvn"] = f(np.stack([pcl(np.asarray(b_kva_norm[l]), 2) for l in range(l0, l0 + L)]))
    wukv = np.asarray(b_w_ukv)[l0:l0 + L].reshape(L, 256, 4, 256)
    m["wukv2"] = f(np.concatenate([wukv[:, :, :, 0:128].reshape(L, 256, 512), wukv[:, :, :, 128:256].reshape(L, 256, 512)], axis=2))
    m["sink"] = f(np.asarray(c_sink)[:L, None, :])
    m["rpbr"] = f(np.asarray(d_rpb)[:L, :, :, ::-1].reshape(L, 60, 31))
    m["wbr"] = f(np.asarray(w_branch)[l0:l0 + L].reshape(L, D, D))
    m["wg"] = f(np.asarray(w_gate)[l0:l0 + L].reshape(L, 4 * D, D))
    bg = np.asarray(b_gate)[l0:l0 + L].reshape(L, 4, 16, 128).transpose(0, 3, 1, 2).reshape(L, 128, 64)
    m["bg"] = f(bg)
    m["wout"] = f(w_out[l0:l0 + L])
    m["wq"] = f(peer_wq[l0:l0 + L])
    m["keysT"] = f(np.asarray(peer_keys)[l0:l0 + L].reshape(L, 16, 128, 128).transpose(0, 3, 1, 2).reshape(L, 128, 2048))
    m["uT"] = f(np.asarray(peer_u)[l0:l0 + L].transpose(0, 2, 1))
    m["v"] = f(peer_v[l0:l0 + L])
    m["fnorm"] = f(pcl(np.asarray(final_norm), 16))
    cs, sn = rope_tables(SEQ)
    m["cs"], m["sn"] = cs, sn
    kk = np.arange(128)[:, None]
    qq = np.arange(128)[None, :]
    m["cmask"] = f(np.concatenate([(qq <= kk), (kk <= qq)], axis=1))
    cq = np.arange(64)[None, :]
    kc = np.arange(64)[:, None]
    cst = np.clip(cq - 8, 0, GRID_W - 16)
    colm = ((kc >= cst) & (kc < cst + 16)).astype(np.float32)
    m["colm"] = f(np.concatenate([colm, colm], axis=0))
    return m


_CACHE = {}


def run_net(SEQ, DEPTH, inputs, dbg=(), stop_after=None, l0=0, xT0=None):
    import os
    key = (SEQ, DEPTH, tuple(dbg), stop_after)
    if key not in _CACHE:
        net = Net(SEQ, DEPTH, dbg)
        net.build(stop_after)
        _CACHE[key] = net
    net = _CACHE[key]
    m = host_inputs(SEQ, DEPTH, l0, xT0, **inputs)
    res = run_bass_kernel_spmd(net.nc, [m], core_ids=[int(os.environ.get('KCORE', '0'))])
    return res.results[0]


def kernel(**inputs):
    SEQ = int(np.asarray(inputs["x"]).shape[1])
    DEPTH = int(np.asarray(inputs["ada_w"]).shape[0])
    xT = None
    r = None
    for l in range(DEPTH):
        r = run_net(SEQ, 1, inputs, dbg=("xT",), l0=l, xT0=xT)
        xT = np.asarray(r["xT"], dtype=np.float32)
    yT = np.asarray(r["yT"], dtype=np.float32)
    return np.ascontiguousarray(yT.T)[None, :, :]
```
